# Optimizing a Trainium2 kernel written in Bass

```python
import math
import jax, jax.numpy as jnp
from jax import lax
import numpy as np

D_MODEL = 1024
BATCH = 8
SEQ = 4096
DEPTH = 2

CHUNK = 64
Q_BLOCK = 128
EPS = 1e-6

D_MIX = D_MODEL
D_SSM = D_MIX // 2
SSM_GROUP = 16
SSM_GROUPS = D_SSM // SSM_GROUP
SSM_STATE = 64
STEP_MIN = 0.001
STEP_MAX = 0.1

MLA_HEADS = 8
D_MLA = D_MIX - D_SSM
MLA_V_DIM = D_MLA // MLA_HEADS
MLA_NOPE = 64
MLA_ROPE = 32
MLA_QK = MLA_NOPE + MLA_ROPE
MLA_Q_RANK = 256
MLA_KV_RANK = 128
ROPE_THETA = 10000.0

D_IN = D_SSM + MLA_Q_RANK + MLA_KV_RANK + MLA_ROPE

FFN_DENSE = 2816
N_EXPERTS = 8
TOP_K = 2
FFN_EXPERT = 3584
N_DENSE = (DEPTH + 1) // 2
N_MOE = DEPTH // 2

kernel_name = "hybrid_s5_mla_moe_chunk_causal"


def rms_norm(x, g):
    xf = x.astype(jnp.float32)
    y = xf * lax.rsqrt(jnp.mean(xf * xf, axis=-1, keepdims=True) + EPS)
    return (y * g.astype(jnp.float32)).astype(x.dtype)


def rope_tables(length):
    inv = 1.0 / (ROPE_THETA ** (jnp.arange(0, MLA_ROPE, 2, dtype=jnp.float32) / MLA_ROPE))
    ang = jnp.arange(length, dtype=jnp.float32)[:, None] * inv[None, :]
    return jnp.cos(ang), jnp.sin(ang)


def apply_rope(x, cos, sin):
    x1, x2 = jnp.split(x, 2, axis=-1)
    c = cos[None, :, None, :].astype(x.dtype)
    s = sin[None, :, None, :].astype(x.dtype)
    return jnp.concatenate([x1 * c - x2 * s, x1 * s + x2 * c], axis=-1)


def swiglu(x, w_gate, w_up, w_down):
    return (jax.nn.silu(x @ w_gate) * (x @ w_up)) @ w_down


def s5_mixer(u, A_re, A_im, log_step, B_re, B_im, C_re, C_im, D, w_glu, b_glu):
    f32 = jnp.float32
    bsz, length, _ = u.shape
    n_chunks = length // CHUNK
    lam = lax.complex(A_re.astype(f32), A_im.astype(f32))
    step = jnp.exp(log_step.astype(f32))[:, None]
    a_bar = jnp.exp(lam * step)
    b_scale = (a_bar - 1.0) / lam
    b_bar = lax.complex(B_re.astype(f32), B_im.astype(f32)) * b_scale[:, :, None]
    c_re = C_re.astype(f32)
    c_im = C_im.astype(f32)
    uf = u.astype(f32).reshape(bsz, n_chunks, CHUNK, SSM_GROUPS, SSM_GROUP)
    uf = jnp.transpose(uf, (1, 2, 0, 3, 4))
    a_seq = jnp.broadcast_to(a_bar, (CHUNK, bsz, SSM_GROUPS, SSM_STATE))

    def combine(earlier, later):
        a1, b1 = earlier
        a2, b2 = later
        return a2 * a1, a2 * b1 + b2

    def chunk_step(state, u_c):
        bu = jnp.einsum('tbgh,gph->tbgp', u_c.astype(jnp.complex64), b_bar)
        a_cum, s_loc = lax.associative_scan(combine, (a_seq, bu), axis=0)
        s = s_loc + a_cum * state[None]
        y = (jnp.einsum('tbgp,ghp->tbgh', s.real, c_re)
             - jnp.einsum('tbgp,ghp->tbgh', s.imag, c_im))
        return s[-1], y

    init = jnp.zeros((bsz, SSM_GROUPS, SSM_STATE), jnp.complex64)
    _, ys = lax.scan(chunk_step, init, uf)
    ys = ys + D.astype(f32) * uf
    y = jnp.transpose(ys, (2, 0, 1, 3, 4)).reshape(bsz, length, D_SSM)
    y = jax.nn.gelu(y)
    y = y * jax.nn.sigmoid(y @ w_glu.astype(f32) + b_glu.astype(f32))
    return y.astype(u.dtype)


def mla_mixer(q_lat, kv_lat, k_rope, q_norm, w_q_up, kv_norm, w_kv_up, q_gain, k_gain, cos, sin):
    bsz, length, _ = q_lat.shape
    q = (rms_norm(q_lat, q_norm) @ w_q_up).reshape(bsz, length, MLA_HEADS, MLA_QK)
    kv = (rms_norm(kv_lat, kv_norm) @ w_kv_up).reshape(bsz, length, MLA_HEADS, MLA_NOPE + MLA_V_DIM)
    k_nope, v = kv[..., :MLA_NOPE], kv[..., MLA_NOPE:]
    k = jnp.concatenate(
        [k_nope, jnp.broadcast_to(k_rope[:, :, None, :], (bsz, length, MLA_HEADS, MLA_ROPE))], axis=-1)
    q = rms_norm(q, q_gain)
    k = rms_norm(k, k_gain)
    q = jnp.concatenate([q[..., :MLA_NOPE], apply_rope(q[..., MLA_NOPE:], cos, sin)], axis=-1)
    k = jnp.concatenate([k[..., :MLA_NOPE], apply_rope(k[..., MLA_NOPE:], cos, sin)], axis=-1)
    scale = MLA_QK ** -0.5
    chunk_id = jnp.arange(length) // CHUNK
    outs = []
    for blk in range(length // Q_BLOCK):
        q0 = blk * Q_BLOCK
        k_end = q0 + Q_BLOCK
        s = jnp.einsum('bqhd,bkhd->bhqk', q[:, q0:k_end], k[:, :k_end],
                       preferred_element_type=jnp.float32) * scale
        mask = chunk_id[q0:k_end, None] >= chunk_id[None, :k_end]
        s = jnp.where(mask[None, None], s, -jnp.inf)
        p = jax.nn.softmax(s, axis=-1)
        outs.append(jnp.einsum('bhqk,bkhd->bqhd', p.astype(v.dtype), v[:, :k_end]))
    return jnp.concatenate(outs, axis=1).reshape(bsz, length, D_MLA)


def moe_swiglu(h, w_router, w_gate, w_up, w_down):
    logits = (h @ w_router).astype(jnp.float32)
    top_val, top_idx = lax.top_k(logits, TOP_K)
    top_w = jax.nn.softmax(top_val, axis=-1)
    gates = jnp.sum(jax.nn.one_hot(top_idx, N_EXPERTS, dtype=jnp.float32) * top_w[..., None], axis=-2)
    out = jnp.zeros_like(h)
    for e in range(N_EXPERTS):
        out = out + gates[..., e:e + 1].astype(h.dtype) * swiglu(h, w_gate[e], w_up[e], w_down[e])
    return out


def setup_inputs(seed: int = 0) -> dict:
    key = jax.random.key(seed)
    ks = jax.random.split(key, 40)
    f32 = jnp.float32

    def nrm(k, shape, scale):
        return jax.random.normal(k, shape, f32) * scale

    def gain(k, shape):
        return 1.0 + 0.02 * jax.random.normal(k, shape, f32)

    G, P, H = SSM_GROUPS, SSM_STATE, SSM_GROUP
    x = jax.random.normal(ks[0], (BATCH, SEQ, D_MODEL), f32)
    attn_norm = gain(ks[1], (DEPTH, D_MODEL))
    w_in = nrm(ks[2], (DEPTH, D_MODEL, D_IN), D_MODEL ** -0.5)
    ssm_A_re = -0.5 + 0.01 * jax.random.normal(ks[3], (DEPTH, G, P), f32)
    ssm_A_im = (math.pi * jnp.arange(P, dtype=f32))[None, None, :] + 0.01 * jax.random.normal(ks[4], (DEPTH, G, P), f32)
    ssm_log_step = jax.random.uniform(ks[5], (DEPTH, G), f32, math.log(STEP_MIN), math.log(STEP_MAX))
    ssm_B_re = nrm(ks[6], (DEPTH, G, P, H), (2 * H) ** -0.5)
    ssm_B_im = nrm(ks[7], (DEPTH, G, P, H), (2 * H) ** -0.5)
    ssm_C_re = nrm(ks[8], (DEPTH, G, H, P), (2 * P) ** -0.5)
    ssm_C_im = nrm(ks[9], (DEPTH, G, H, P), (2 * P) ** -0.5)
    ssm_D = nrm(ks[10], (DEPTH, G, H), 1.0)
    ssm_w_glu = nrm(ks[11], (DEPTH, D_SSM, D_SSM), D_SSM ** -0.5)
    ssm_b_glu = nrm(ks[12], (DEPTH, D_SSM), 0.01)
    mla_q_norm = gain(ks[13], (DEPTH, MLA_Q_RANK))
    mla_w_q_up = nrm(ks[14], (DEPTH, MLA_Q_RANK, MLA_HEADS * MLA_QK), MLA_Q_RANK ** -0.5)
    mla_kv_norm = gain(ks[15], (DEPTH, MLA_KV_RANK))
    mla_w_kv_up = nrm(ks[16], (DEPTH, MLA_KV_RANK, MLA_HEADS * (MLA_NOPE + MLA_V_DIM)), MLA_KV_RANK ** -0.5)
    mla_q_gain = gain(ks[17], (DEPTH, MLA_QK))
    mla_k_gain = gain(ks[18], (DEPTH, MLA_QK))
    ssm_out_norm = gain(ks[19], (DEPTH, D_SSM))
    mla_out_norm = gain(ks[20], (DEPTH, D_MLA))
    w_out = nrm(ks[21], (DEPTH, D_MIX, D_MODEL), D_MIX ** -0.5)
    ffn_norm = gain(ks[22], (DEPTH, D_MODEL))
    dense_w_gate = nrm(ks[23], (N_DENSE, D_MODEL, FFN_DENSE), D_MODEL ** -0.5)
    dense_w_up = nrm(ks[24], (N_DENSE, D_MODEL, FFN_DENSE), D_MODEL ** -0.5)
    dense_w_down = nrm(ks[25], (N_DENSE, FFN_DENSE, D_MODEL), FFN_DENSE ** -0.5)
    moe_w_router = nrm(ks[26], (N_MOE, D_MODEL, N_EXPERTS), D_MODEL ** -0.5)
    moe_w_gate = nrm(ks[27], (N_MOE, N_EXPERTS, D_MODEL, FFN_EXPERT), D_MODEL ** -0.5)
    moe_w_up = nrm(ks[28], (N_MOE, N_EXPERTS, D_MODEL, FFN_EXPERT), D_MODEL ** -0.5)
    moe_w_down = nrm(ks[29], (N_MOE, N_EXPERTS, FFN_EXPERT, D_MODEL), FFN_EXPERT ** -0.5)
    return {
        "x": x, "attn_norm": attn_norm, "w_in": w_in,
        "ssm_A_re": ssm_A_re, "ssm_A_im": ssm_A_im, "ssm_log_step": ssm_log_step,
        "ssm_B_re": ssm_B_re, "ssm_B_im": ssm_B_im, "ssm_C_re": ssm_C_re, "ssm_C_im": ssm_C_im,
        "ssm_D": ssm_D, "ssm_w_glu": ssm_w_glu, "ssm_b_glu": ssm_b_glu,
        "mla_q_norm": mla_q_norm, "mla_w_q_up": mla_w_q_up, "mla_kv_norm": mla_kv_norm,
        "mla_w_kv_up": mla_w_kv_up, "mla_q_gain": mla_q_gain, "mla_k_gain": mla_k_gain,
        "ssm_out_norm": ssm_out_norm, "mla_out_norm": mla_out_norm, "w_out": w_out,
        "ffn_norm": ffn_norm, "dense_w_gate": dense_w_gate, "dense_w_up": dense_w_up,
        "dense_w_down": dense_w_down, "moe_w_router": moe_w_router, "moe_w_gate": moe_w_gate,
        "moe_w_up": moe_w_up, "moe_w_down": moe_w_down,
    }


def reference(x, attn_norm, w_in, ssm_A_re, ssm_A_im, ssm_log_step, ssm_B_re, ssm_B_im,
              ssm_C_re, ssm_C_im, ssm_D, ssm_w_glu, ssm_b_glu, mla_q_norm, mla_w_q_up,
              mla_kv_norm, mla_w_kv_up, mla_q_gain, mla_k_gain, ssm_out_norm, mla_out_norm,
              w_out, ffn_norm, dense_w_gate, dense_w_up, dense_w_down, moe_w_router,
              moe_w_gate, moe_w_up, moe_w_down):
    cos, sin = rope_tables(x.shape[1])
    split_at = [D_SSM, D_SSM + MLA_Q_RANK, D_SSM + MLA_Q_RANK + MLA_KV_RANK]
    h = x
    for layer in range(DEPTH):
        z = rms_norm(h, attn_norm[layer])
        proj = z @ w_in[layer]
        u, q_lat, kv_lat, k_rope = jnp.split(proj, split_at, axis=-1)
        y_ssm = s5_mixer(u, ssm_A_re[layer], ssm_A_im[layer], ssm_log_step[layer],
                         ssm_B_re[layer], ssm_B_im[layer], ssm_C_re[layer], ssm_C_im[layer],
                         ssm_D[layer], ssm_w_glu[layer], ssm_b_glu[layer])
        y_mla = mla_mixer(q_lat, kv_lat, k_rope, mla_q_norm[layer], mla_w_q_up[layer],
                          mla_kv_norm[layer], mla_w_kv_up[layer], mla_q_gain[layer],
                          mla_k_gain[layer], cos, sin)
        mixed = jnp.concatenate([rms_norm(y_ssm, ssm_out_norm[layer]),
                                 rms_norm(y_mla, mla_out_norm[layer])], axis=-1)
        h = h + mixed @ w_out[layer]
        z = rms_norm(h, ffn_norm[layer])
        if layer % 2 == 0:
            i = layer // 2
            h = h + swiglu(z, dense_w_gate[i], dense_w_up[i], dense_w_down[i])
        else:
            i = layer // 2
            h = h + moe_swiglu(z, moe_w_router[i], moe_w_gate[i], moe_w_up[i], moe_w_down[i])
    return h
```

```python
import threading
import numpy as np
from contextlib import ExitStack
import concourse.bass as bass
import concourse.mybir as mybir
from concourse.bass_utils import run_bass_kernel_spmd

F32 = mybir.dt.float32
BF16 = mybir.dt.bfloat16
AF = mybir.ActivationFunctionType
ALU = mybir.AluOpType
AX = mybir.AxisListType

T = 4096
D = 1024
TS = 512
NT = T // TS
COOP_DEPTH = 2
NCH = 32
PI = float(np.pi)
EPS = 1e-6

CP = {}
_o = 0
for _n, _w in [("attn_norm", 8), ("ffn_norm", 8), ("q_norm", 2), ("kv_norm", 1), ("q_gain", 1),
               ("k_gain", 1), ("ssm_out_norm", 4), ("mla_out_norm", 8), ("b_glu", 4), ("ssm_D", 4),
               ("fA_re", 16), ("fA_im", 16), ("fLs", 16)]:
    CP[_n] = (_o, _w)
    _o += _w
CPL = _o
C_J1 = 2 * CPL
NCOLS = C_J1 + 1


class V:
    __slots__ = ("b", "ap")

    def __init__(s, b, ap):
        s.b = b
        s.ap = ap


class Buf:
    def __init__(s, name, t):
        s.name = name
        s.t = t
        s.w = None
        s.r = {}
        s.dsem = None
        s.dcnt = 0

    def __getitem__(s, idx):
        return V(s, s.t[idx])

    def v(s, ap):
        return V(s, ap)


class Coop:
    def __init__(s):
        s.cv = threading.Condition()
        s.turn = None
        s.live = []
        s.err = None
        s.workers = set()

    def hook(s):
        me = threading.get_ident()
        if me not in s.workers:
            return
        with s.cv:
            if len(s.live) < 2:
                return
            idx = s.live.index(me)
            s.turn = s.live[(idx + 1) % len(s.live)]
            s.cv.notify_all()
            while s.turn != me:
                s.cv.wait()

    def run(s, fns, depth=2):
        pending = list(fns)
        done = threading.Event()
        threads = []

        def spawn(fn):
            t = threading.Thread(target=worker, args=(fn,))
            t.start()
            s.live.append(t.ident)
            s.workers.add(t.ident)
            threads.append(t)

        def worker(fn):
            me = threading.get_ident()
            with s.cv:
                while s.turn != me:
                    s.cv.wait()
            try:
                if s.err is None:
                    fn()
            except BaseException as e:
                s.err = e
            with s.cv:
                idx = s.live.index(me)
                s.live.remove(me)
                s.workers.discard(me)
                if pending:
                    spawn(pending.pop(0))
                if s.live:
                    s.turn = s.live[idx % len(s.live)]
                else:
                    s.turn = None
                    done.set()
                s.cv.notify_all()

        with s.cv:
            for _ in range(min(depth, len(pending))):
                spawn(pending.pop(0))
            s.turn = s.live[0]
            s.cv.notify_all()
        done.wait()
        for t in threads:
            t.join()
        if s.err is not None:
            e, s.err = s.err, None
            raise e


def interleave(fns, depth=2):
    pending = list(fns)
    live = []
    while pending or live:
        while pending and len(live) < depth:
            live.append(pending.pop(0)())
        for g in list(live):
            try:
                next(g)
            except StopIteration:
                live.remove(g)


class Banks:
    def __init__(s, banks):
        s.b = list(banks)
        s.i = -1

    def next(s):
        s.i += 1
        return s.b[s.i % len(s.b)]


class K:
    def __init__(s):
        s.nc = bass.Bass("TRN2", target_bir_lowering=False)
        nc = s.nc
        s.es = ExitStack()
        s.E = {'pe': nc.tensor, 'act': nc.scalar, 'dve': nc.vector, 'pool': nc.gpsimd, 'sp': nc.sync}
        s.sem = {e: s.es.enter_context(nc.semaphore("S_" + e)) for e in ('pe', 'act', 'dve', 'pool')}
        s.cnt = {e: 0 for e in s.sem}
        s.known = {e: {} for e in s.E}
        s.pend = {e: ([], []) for e in s.sem}
        s.dmasems = {}
        s.nsem = 4
        s.uid = 0
        s.coop = Coop()
        s.stacks = []
        s.stack_bufs = []
        s.freesems = []

    def push(s):
        st = ExitStack()
        s.stacks.append(st)
        s.stack_bufs.append([])
        return st

    def pop(s):
        s.barrier()
        st = s.stacks.pop()
        for b in s.stack_bufs.pop():
            if b.dsem is not None:
                s.freesems.append((b.dsem, b.dcnt))
                b.dsem = None
        st.close()

    def sb(s, name, shape, dt):
        s.uid += 1
        t = s.stacks[-1].enter_context(s.nc.sbuf_tensor(f"{name}_{s.uid}", list(shape), dt))
        b = Buf(name, t)
        s.stack_bufs[-1].append(b)
        return b

    def ps(s, name, shape, dt=F32):
        s.uid += 1
        t = s.stacks[-1].enter_context(s.nc.psum_tensor(f"{name}_{s.uid}", list(shape), dt))
        return Buf(name, t)

    def dram(s, name, shape, dt, kind):
        t = s.nc.dram_tensor(name, list(shape), dt, kind=kind).ap()
        return Buf(name, t)

    def newsem(s, name):
        s.nsem += 1
        s.uid += 1
        return s.es.enter_context(s.nc.semaphore(f"D_{name}_{s.uid}"))

    def _waits(s, eng, reads, writes):
        ev = {}
        for b in reads:
            if b.w is not None:
                k, v_ = b.w
                if ev.get(k, 0) < v_:
                    ev[k] = v_
        for b in writes:
            if b.w is not None:
                k, v_ = b.w
                if ev.get(k, 0) < v_:
                    ev[k] = v_
            for k, v_ in b.r.items():
                if ev.get(k, 0) < v_:
                    ev[k] = v_
        kn = s.known[eng]
        E = s.E[eng]
        for k, v_ in ev.items():
            if eng == 'pe' and k is s.sem['pe']:
                continue
            if kn.get(k, 0) >= v_:
                continue
            E.wait_ge(k, v_)
            kn[k] = v_

    def _commit(s, ev, reads, writes):
        k, v_ = ev
        for b in reads:
            if b.r.get(k, 0) < v_:
                b.r[k] = v_
        for b in writes:
            b.w = ev
            b.r = {}

    def op(s, eng, fn, ins, outs, inc=True):
        reads = [x.b for x in ins]
        writes = [x.b for x in outs]
        s._waits(eng, reads, writes)
        I = fn(s.E[eng])
        if inc:
            s.cnt[eng] += 1
            I.then_inc(s.sem[eng], 1)
            ev = (s.sem[eng], s.cnt[eng])
            pr, pw = s.pend[eng]
            s._commit(ev, reads + pr, writes + pw)
            s.pend[eng] = ([], [])
        else:
            pr, pw = s.pend[eng]
            pr.extend(reads)
            pw.extend(writes)
        return I

    def dma(s, q, out, in_, **kw):
        reads = [in_.b]
        writes = [out.b]
        d = out.b
        saved = d.w
        if d.w is not None and d.dsem is not None and d.w[0] is d.dsem:
            d.w = None
        s._waits(q, reads, writes)
        d.w = saved
        if d.dsem is None:
            if s.freesems:
                d.dsem, d.dcnt = s.freesems.pop()
            else:
                d.dsem = s.newsem(d.name)
        d.dcnt += 1
        I = s.E[q].dma_start(out=out.ap, in_=in_.ap, **kw)
        I.then_inc(d.dsem, 16)
        ev = (d.dsem, 16 * d.dcnt)
        s.dmasems[d.dsem] = 16 * d.dcnt
        s._commit(ev, reads, writes)

    def barrier(s):
        for e in s.E:
            kn = s.known[e]
            for e2 in s.sem:
                k, v_ = s.sem[e2], s.cnt[e2]
                if v_ > 0 and kn.get(k, 0) < v_:
                    s.E[e].wait_ge(k, v_)
                    kn[k] = v_
            for k, v_ in s.dmasems.items():
                if kn.get(k, 0) < v_:
                    s.E[e].wait_ge(k, v_)
                    kn[k] = v_

    def mm(s, out, lhsT, rhs, start=True, stop=True, inc=None):
        if inc is None:
            inc = stop
        return s.op('pe', lambda e: e.matmul(out.ap, lhsT=lhsT.ap, rhs=rhs.ap, start=start, stop=stop),
                    [lhsT, rhs], [out], inc=inc)

    def act(s, out, in_, func, bias=None, scale=None, eng='act'):
        ins = [in_]
        kw = {}
        if bias is not None:
            if isinstance(bias, V):
                ins.append(bias)
                kw['bias'] = bias.ap
            else:
                kw['bias'] = bias
        if scale is not None:
            if isinstance(scale, V):
                ins.append(scale)
                kw['scale'] = scale.ap
            else:
                kw['scale'] = scale
        return s.op(eng, lambda e: e.activation(out=out.ap, in_=in_.ap, func=func, **kw), ins, [out])

    def tt(s, out, in0, in1, op, eng='dve'):
        return s.op(eng, lambda e: e.tensor_tensor(out=out.ap, in0=in0.ap, in1=in1.ap, op=op), [in0, in1], [out])

    def ts(s, out, in0, s1, op0, s2=None, op1=None, eng='dve'):
        ins = [in0]
        a1 = s1
        if isinstance(s1, V):
            ins.append(s1)
            a1 = s1.ap
        a2 = s2
        if isinstance(s2, V):
            ins.append(s2)
            a2 = s2.ap
        if op1 is None:
            return s.op(eng, lambda e: e.tensor_scalar(out=out.ap, in0=in0.ap, scalar1=a1, scalar2=None, op0=op0), ins, [out])
        return s.op(eng, lambda e: e.tensor_scalar(out=out.ap, in0=in0.ap, scalar1=a1, scalar2=a2, op0=op0, op1=op1), ins, [out])

    def stt(s, out, in0, sc, in1, op0, op1, eng='dve'):
        ins = [in0, in1]
        a = sc
        if isinstance(sc, V):
            ins.append(sc)
            a = sc.ap
        return s.op(eng, lambda e: e.scalar_tensor_tensor(out=out.ap, in0=in0.ap, scalar=a, in1=in1.ap, op0=op0, op1=op1), ins, [out])

    def copy(s, out, in_, eng='dve'):
        if eng == 'act':
            return s.act(out, in_, AF.Copy)
        return s.op(eng, lambda e: e.tensor_copy(out=out.ap, in_=in_.ap), [in_], [out])

    def memset(s, out, val, eng='dve'):
        return s.op(eng, lambda e: e.memset(out.ap, val), [], [out])


def bc(v, shape):
    return V(v.b, v.ap.to_broadcast(list(shape)))


def build(dbg=None, stop_after=None):
    k = K()
    nc = k.nc
    dbg = dbg or {}
    di = {}

    def inp(name, shape):
        di[name] = k.dram(name, shape, F32, "ExternalInput")
        return di[name]

    xT = inp("xT", [D, T])
    w_in = inp("w_in", [2, D, 928])
    w_glu = inp("ssm_w_glu", [2, 512, 512])
    w_q_up = inp("mla_w_q_up", [2, 256, 768])
    w_kv_up = inp("mla_w_kv_up", [2, 128, 1024])
    w_out = inp("w_out", [2, D, D])
    dwg = inp("dense_w_gate", [1, D, 2816])
    dwu = inp("dense_w_up", [1, D, 2816])
    dwd = inp("dense_w_down", [1, 2816, D])
    mwr = inp("moe_w_router", [1, D, 8])
    mwg = inp("moe_w_gate", [1, 8, D, 3584])
    mwu = inp("moe_w_up", [1, 8, D, 3584])
    mwd = inp("moe_w_down", [1, 8, 3584, D])
    cols_d = inp("cols", [128, NCOLS])
    rowA_re = inp("rowA_re", [2, 128, 4096])
    rowA_im = inp("rowA_im", [2, 128, 4096])
    rowLs = inp("rowLs", [2, 128, 4096])
    bbA_re = inp("bbA_re", [2, 128, 2048])
    bbA_im = inp("bbA_im", [2, 128, 2048])
    bbLs = inp("bbLs", [2, 128, 2048])
    BRd = inp("BR", [2, 128, 2048])
    BId = inp("BI", [2, 128, 2048])
    Cbd_d = inp("Cbd", [2, 128, 4096])
    consts_d = inp("consts", [128, 640])
    ropeC_d = inp("ropeC", [96, T])
    ropeS_d = inp("ropeS", [96, T])

    outT = k.dram("outT", [D, T], F32, "ExternalOutput")
    hscr = [k.dram("hA", [D, T], F32, "Internal"), k.dram("hB", [D, T], F32, "Internal")]
    oscr = k.dram("oscr", [8, 64, T], F32, "Internal")
    for nm, shp in dbg.items():
        if not nm.startswith("_"):
            di["dbg_" + nm] = k.dram("dbg_" + nm, shp, F32, "ExternalOutput")

    def htiles(buf):
        return [Buf(f"{buf.name}_t{n}", buf.t) for n in range(NT)]
    H_x = htiles(xT)
    H_A = htiles(hscr[0])
    H_B = htiles(hscr[1])
    H_O = htiles(outT)

    O_state = {"O_h": [Buf(f"o{h}", oscr.t) for h in range(8)]}

    def hview(tl, n):
        return V(tl[n], tl[n].t.rearrange("(c p) t -> p c t", p=128)[:, :, n * TS:(n + 1) * TS])

    k.push()
    cols = k.sb("cols", [128, NCOLS], F32)
    k.dma('sp', cols[:, :], di["cols"][:, :])
    cst = k.sb("cst", [128, 640], F32)
    k.dma('sp', cst[:, :], di["consts"][:, :])
    cstb = k.sb("cstb", [128, 640], BF16)
    k.copy(cstb[:, :], cst[:, :])
    ident_f = cst[:, 0:128]
    tri_b = cstb[:, 128:256]
    t1row = cst[:, 256:384]
    swapT_b = cstb[0:96, 384:480]
    ones_b = cstb[:, 512:640]
    psb = [k.ps(f"ps{i}", [128, 512]) for i in range(8)]
    pctr = [0]

    def nps():
        pctr[0] += 1
        return psb[pctr[0] % 6]

    def col(l, name, j=0, rows=128):
        o, w = CP[name]
        c = l * CPL + o + j
        return cols[0:rows, c:c + 1]

    def dump(name, view_fn):
        if name in dbg:
            view_fn(di["dbg_" + name])

    def rstd_from_ps(ps_v, out_v, n_feat):
        k.ts(out_v, ps_v, 1.0 / n_feat, ALU.mult, EPS, ALU.add)
        k.act(out_v, out_v, AF.Ln)
        k.act(out_v, out_v, AF.Exp, scale=-0.5)

    def sincos(ang, sin_out, cos_out, tmp, ti):
        for shift, outv in ((0.0, sin_out), (0.5 * PI, cos_out)):
            k.ts(tmp, ang, shift, ALU.add, 1.0 / (2 * PI), ALU.mult)
            k.copy(ti, tmp)
            k.copy(tmp, ti)
            k.stt(tmp, tmp, -2 * PI, ang, ALU.mult, ALU.add)
            if shift != 0.0:
                k.ts(tmp, tmp, shift, ALU.add)
            k.ts(outv, tmp, PI, ALU.is_gt, -2 * PI, ALU.mult)
            k.tt(tmp, tmp, outv, ALU.add)
            k.act(outv, tmp, AF.Sin)

    def mixer_layer(l, Hin, Hmid):
        k.push()
        uT = k.sb("uT", [128, 4, T], BF16)
        uT_t = [Buf(f"uT{n}", uT.t) for n in range(NT)]
        k.push()
        qn = k.sb("qn", [128, 2, T], BF16)
        kvn = k.sb("kvn", [128, T], BF16)
        krope = k.sb("krope", [96, T], BF16)
        qn_t = [Buf(f"qn{n}", qn.t) for n in range(NT)]
        kvn_t = [Buf(f"kvn{n}", kvn.t) for n in range(NT)]
        kr_t = [Buf(f"kr{n}", krope.t) for n in range(NT)]

        if dbg.get("_only_tab"):
            k.pop()
            ssm_phase(l, uT, uT_t, Hmid, Hmid)
            k.pop()
            return False
        k.push()
        winb = k.sb("winb", [128, 8, 928], BF16)
        k.dma('pool', winb[:, :, :], V(w_in, w_in.t[l].rearrange("(c p) n -> p c n", p=128)))
        hbuf = [k.sb("hbuf", [128, 8, TS], F32) for _ in range(2)]
        sq = [[k.sb("sq", [128, TS], BF16) for _ in range(2)] for _ in range(2)]
        rstd2 = [k.sb("rstd", [128, TS], F32) for _ in range(2)]
        zb = [k.sb("zb", [128, 8, TS], BF16) for _ in range(2)]
        sqq2 = [[k.sb("sqq", [128, TS], BF16) for _ in range(2)] for _ in range(2)]
        rq2 = [k.sb("rq", [128, TS], F32) for _ in range(2)]

        def p1_body(n):
            par = n % 2
            bk = Banks(psb[3 * par:3 * par + 3])
            sl = slice(n * TS, (n + 1) * TS)
            hb = hbuf[par]
            z = zb[par]
            rstd, rq, sqq = rstd2[par], rq2[par], sqq2[par]
            k.dma('sp', hb[:, :, :], hview(Hin, n))
            yield
            pss = bk.next()
            for c in range(8):
                sqc = sq[par][c % 2]
                k.act(sqc[:, :], hb[:, c, :], AF.Square)
                k.mm(pss[:, :], ones_b, sqc[:, :], start=(c == 0), stop=(c == 7), inc=True)
                if c % 2 == 1:
                    yield
            rstd_from_ps(pss[:, :], rstd[:, :], D)
            yield
            for c in range(8):
                k.stt(z[:, c, :], hb[:, c, :], col(l, "attn_norm", c), rstd[:, :], ALU.mult, ALU.mult)
                if c % 4 == 3:
                    yield
            for m in range(4):
                p_ = bk.next()
                for c in range(8):
                    k.mm(p_[:, :], winb[:, c, m * 128:(m + 1) * 128], z[:, c, :], start=(c == 0), stop=(c == 7))
                if m % 2 == 0:
                    k.copy(V(uT_t[n], uT.t[:, m, sl]), p_[:, :], eng='act')
                else:
                    k.copy(V(uT_t[n], uT.t[:, m, sl]), p_[:, :], eng='dve')
                yield
            pq = [bk.next(), bk.next()]
            for m in range(2):
                for c in range(8):
                    k.mm(pq[m][:, :], winb[:, c, 512 + m * 128:512 + (m + 1) * 128], z[:, c, :], start=(c == 0), stop=(c == 7))
                yield
            pqs = bk.next()
            for m in range(2):
                k.act(sqq[m][:, :], pq[m][:, :], AF.Square)
                k.mm(pqs[:, :], ones_b, sqq[m][:, :], start=(m == 0), stop=(m == 1), inc=True)
            yield
            rstd_from_ps(pqs[:, :], rq[:, :], 256)
            yield
            for m in range(2):
                k.stt(V(qn_t[n], qn.t[:, m, sl]), pq[m][:, :], col(l, "q_norm", m), rq[:, :], ALU.mult, ALU.mult)
            yield
            pk = bk.next()
            for c in range(8):
                k.mm(pk[:, :], winb[:, c, 768:896], z[:, c, :], start=(c == 0), stop=(c == 7))
            yield
            pks = bk.next()
            k.act(sqq[0][:, :], pk[:, :], AF.Square)
            k.mm(pks[:, :], ones_b, sqq[0][:, :], start=True, stop=True)
            yield
            rstd_from_ps(pks[:, :], rq[:, :], 128)
            yield
            k.stt(V(kvn_t[n], kvn.t[:, sl]), pk[:, :], col(l, "kv_norm", 0), rq[:, :], ALU.mult, ALU.mult)
            yield
            pr = bk.next()
            for c in range(8):
                k.mm(pr[0:96, :], winb[:, c, 832:928], z[:, c, :], start=(c == 0), stop=(c == 7))
            k.copy(V(kr_t[n], krope.t[64:96, sl]), pr[64:96, :], eng='act')

        interleave([(lambda n=n: p1_body(n)) for n in range(NT)], depth=COOP_DEPTH)
        k.pop()
        if "p1" in dbg and stop_after == "p1" and l == dbg.get("_layer", 0):
            d_ = di["dbg_p1"]
            for m in range(4):
                k.dma('sp', V(d_, d_.t[m * 128:(m + 1) * 128, :]), V(uT, uT.t[:, m, :]), allow_cast=True) if False else None
            tmpd = k.sb("tmpd", [128, T], F32)
            for m in range(4):
                k.copy(tmpd[:, :], V(uT, uT.t[:, m, :]))
                k.dma('sp', V(d_, d_.t[m * 128:(m + 1) * 128, :]), tmpd[:, :])
            for m in range(2):
                k.copy(tmpd[:, :], V(qn, qn.t[:, m, :]))
                k.dma('sp', V(d_, d_.t[512 + m * 128:512 + (m + 1) * 128, :]), tmpd[:, :])
            k.copy(tmpd[:, :], V(kvn, kvn.t[:, :]))
            k.dma('sp', V(d_, d_.t[768:896, :]), tmpd[:, :])
            k.dma('pool', V(d_, d_.t[896:928, :]), V(krope, krope.t[64:96, :]))
        if stop_after == "p1":
            k.pop()
            k.pop()
            return False
        mla_phase(l, qn, qn_t, kvn, kvn_t, krope, kr_t)
        k.pop()
        if stop_after == "mla":
            k.pop()
            return False
        mla_out(l, Hin, Hmid)
        if stop_after == "mlaout":
            k.pop()
            return True
        ssm_phase(l, uT, uT_t, Hmid, Hmid)
        k.pop()
        return True

    def ssm_phase(l, uT, uT_t, Hin, Hmid):
        k.push()
        yss = k.sb("yss", [128, 4, T], BF16)
        yss_c = [Buf(f"yss{c}", yss.t) for c in range(NCH)]
        k.push()
        Bbd = k.sb("Bbd", [128, 4, 512], BF16)
        Cbd = k.sb("Cbd", [128, 16, 2, 128], BF16)
        WA = k.sb("WA", [128, 4096], F32)
        WB = k.sb("WB", [128, 4096], F32)
        Pre = k.sb("Pre", [128, 16, 128], F32)
        Pim = k.sb("Pim", [128, 16, 128], F32)
        Preb = k.sb("Preb", [128, 16, 128], BF16)
        Pimb = k.sb("Pimb", [128, 16, 128], BF16)
        nPimb = k.sb("nPimb", [128, 16, 128], BF16)
        k.dma('pool', Cbd[:, :, :, :], V(Cbd_d, Cbd_d.t[l].rearrange("p (a b c) -> p a b c", a=16, b=2)))
        k.push()
        tA = k.sb("tA", [128, 4096], F32)
        tB = k.sb("tB", [128, 4096], F32)
        tC = k.sb("tC", [128, 4096], F32)
        tD = WA
        tE = WB
        tI = k.sb("tI", [128, 4096], mybir.dt.int32)
        j1 = cols[:, C_J1:C_J1 + 1]
        h2 = slice(0, 2048)
        k.dma('sp', tA[:, h2], V(bbLs, bbLs.t[l]))
        k.dma('sp', tB[:, h2], V(bbA_re, bbA_re.t[l]))
        k.dma('sp', tC[:, h2], V(bbA_im, bbA_im.t[l]))
        k.act(tA[:, h2], tA[:, h2], AF.Exp)
        k.tt(tD[:, h2], tB[:, h2], tA[:, h2], ALU.mult)
        k.tt(tE[:, h2], tC[:, h2], tA[:, h2], ALU.mult)
        k.act(tD[:, h2], tD[:, h2], AF.Exp)
        h3 = slice(2048, 4096)
        sincos(tE[:, h2], tA[:, h3], tD[:, h3], tA[:, h2], tI[:, h2])
        k.tt(tD[:, h3], tD[:, h3], tD[:, h2], ALU.mult)
        k.ts(tD[:, h3], tD[:, h3], -1.0, ALU.add)
        k.tt(tA[:, h3], tA[:, h3], tD[:, h2], ALU.mult)
        k.tt(tA[:, h2], tB[:, h2], tB[:, h2], ALU.mult)
        k.tt(tD[:, h2], tC[:, h2], tC[:, h2], ALU.mult)
        k.tt(tA[:, h2], tA[:, h2], tD[:, h2], ALU.add)
        k.op('dve', lambda e: e.reciprocal(out=tA.t[:, h2], in_=tA.t[:, h2]), [tA[:, h2]], [tA[:, h2]])
        k.tt(tE[:, h2], tD[:, h3], tB[:, h2], ALU.mult)
        k.tt(tE[:, h3], tA[:, h3], tC[:, h2], ALU.mult)
        k.tt(tE[:, h2], tE[:, h2], tE[:, h3], ALU.add)
        k.tt(tE[:, h2], tE[:, h2], tA[:, h2], ALU.mult)
        k.tt(tE[:, h3], tA[:, h3], tB[:, h2], ALU.mult)
        k.tt(tD[:, h2], tD[:, h3], tC[:, h2], ALU.mult)
        k.tt(tE[:, h3], tE[:, h3], tD[:, h2], ALU.subtract)
        k.tt(tE[:, h3], tE[:, h3], tA[:, h2], ALU.mult)
        def slot(buf, hs, r):
            a = buf.t[:, hs].rearrange("p (a r c) -> p a r c", r=2, c=128)
            return V(buf, a[:, :, r, :])
        k.dma('sp', tB[:, h2], V(BRd, BRd.t[l]))
        k.dma('sp', tC[:, h2], V(BId, BId.t[l]))
        k.tt(slot(tA, h3, 0), slot(tB, h2, 0), slot(tE, h2, 0), ALU.mult)
        k.tt(slot(tD, h3, 0), slot(tC, h2, 0), slot(tE, h3, 0), ALU.mult)
        k.tt(slot(tA, h3, 0), slot(tA, h3, 0), slot(tD, h3, 0), ALU.subtract)
        k.tt(slot(tA, h3, 1), slot(tB, h2, 1), slot(tE, h3, 1), ALU.mult)
        k.tt(slot(tD, h3, 1), slot(tC, h2, 1), slot(tE, h2, 1), ALU.mult)
        k.tt(slot(tA, h3, 1), slot(tA, h3, 1), slot(tD, h3, 1), ALU.add)
        k.copy(V(Bbd, Bbd.t[:, :, :].rearrange("p a b -> p (a b)")), tA[:, h3])
        fo = CP["fA_re"][0] + l * CPL
        fi = CP["fA_im"][0] + l * CPL
        fl = CP["fLs"][0] + l * CPL
        st16 = tB[:, 0:16]
        r16 = tB[:, 16:32]
        th16 = tB[:, 32:48]
        k.act(st16, cols[:, fl:fl + 16], AF.Exp)
        k.tt(r16, cols[:, fo:fo + 16], st16, ALU.mult)
        k.tt(th16, cols[:, fi:fi + 16], st16, ALU.mult)
        if "ssmtab2" in dbg:
            k.dma('sp', V(di["dbg_ssmtab2"], di["dbg_ssmtab2"].t[:, 0:48]), tB[:, 0:48])
            k.dma('sp', V(di["dbg_ssmtab2"], di["dbg_ssmtab2"].t[:, 64:64 + NCOLS]), cols[:, :])
        PR = tC.t[:, 0:2048].rearrange("p (a t) -> p a t", t=128)
        PA = tD.t[:, 0:2048].rearrange("p (a t) -> p a t", t=128)
        t1b = V(cst, t1row.ap.unsqueeze(1).to_broadcast([128, 16, 128]))
        k.tt(V(tC, PR), t1b, V(tB, r16.ap.unsqueeze(2).to_broadcast([128, 16, 128])), ALU.mult)
        k.act(tC[:, 0:2048], tC[:, 0:2048], AF.Exp)
        k.tt(V(tD, PA), t1b, V(tB, th16.ap.unsqueeze(2).to_broadcast([128, 16, 128])), ALU.mult)
        sincos(tD[:, 0:2048], tE[:, 0:2048], tE[:, 2048:4096], tA[:, 0:2048], tI[:, 0:2048])
        k.tt(V(Pre, Pre.t[:, :, :].rearrange("p a t -> p (a t)")), tC[:, 0:2048], tE[:, 2048:4096], ALU.mult)
        k.tt(V(Pim, Pim.t[:, :, :].rearrange("p a t -> p (a t)")), tC[:, 0:2048], tE[:, 0:2048], ALU.mult)
        fl2 = lambda b_: V(b_, b_.t[:, :, :].rearrange("p a t -> p (a t)"))
        k.ts(fl2(nPimb), fl2(Pim), -1.0, ALU.mult)
        k.copy(fl2(Preb), fl2(Pre))
        k.copy(fl2(Pimb), fl2(Pim))
        k.dma('sp', tA[:, :], V(rowLs, rowLs.t[l]))
        k.dma('sp', tB[:, :], V(rowA_re, rowA_re.t[l]))
        k.dma('sp', tC[:, :], V(rowA_im, rowA_im.t[l]))
        k.act(tA[:, :], tA[:, :], AF.Exp)
        k.tt(tB[:, :], tB[:, :], tA[:, :], ALU.mult)
        k.tt(tC[:, :], tC[:, :], tA[:, :], ALU.mult)
        k.ts(tB[:, :], tB[:, :], j1, ALU.mult, -1.0, ALU.mult)
        k.act(tB[:, :], tB[:, :], AF.Exp)
        k.ts(tC[:, :], tC[:, :], j1, ALU.mult)
        sincos(tC[:, :], WB[:, :], WA[:, :], tA[:, :], tI[:, :])
        k.tt(WA[:, :], tB[:, :], WA[:, :], ALU.mult)
        k.tt(WB[:, :], tB[:, :], WB[:, :], ALU.mult)
        wb4 = WB.t[:, :].rearrange("p (a r c) -> p a r c", r=2, c=128)
        k.ts(V(WB, wb4[:, :, 1, :]), V(WB, wb4[:, :, 1, :]), -1.0, ALU.mult)
        if "ssmtab" in dbg:
            d_ = di["dbg_ssmtab"]
            k.dma('sp', V(d_, d_.t[:, 0:4096]), WA[:, :])
            k.dma('sp', V(d_, d_.t[:, 4096:8192]), WB[:, :])
            k.dma('sp', V(d_, d_.t[:, 8192:10240]), V(Pre, Pre.t[:, :, :].rearrange("p a t -> p (a t)")))
            k.dma('sp', V(d_, d_.t[:, 10240:12288]), V(Pim, Pim.t[:, :, :].rearrange("p a t -> p (a t)")))
            k.copy(tA[:, 0:2048], V(Bbd, Bbd.t[:, :, :].rearrange("p a b -> p (a b)")))
            k.dma('sp', V(d_, d_.t[:, 12288:14336]), tA[:, 0:2048])
        k.pop()
        if stop_after == "ssmtab":
            k.pop()
            k.pop()
            return
        k.push()
        T1 = [k.sb("T1", [128, 4096], BF16) for _ in range(2)]
        T2 = [k.sb("T2", [128, 4096], BF16) for _ in range(2)]
        zr = k.sb("zr", [128, 1024], BF16)
        zin = k.sb("zin", [128, 1024], BF16)
        czr = k.sb("czr", [128, 8], F32)
        czi = k.sb("czi", [128, 8], F32)
        Q1 = [k.sb("Q1", [128, 8, 128], BF16) for _ in range(2)]
        Q2 = [k.sb("Q2", [128, 8, 128], BF16) for _ in range(2)]
        Q3 = [k.sb("Q3", [128, 8, 128], BF16) for _ in range(2)]
        Q4 = [k.sb("Q4", [128, 8, 128], BF16) for _ in range(2)]
        u1 = k.sb("u1", [128, 8], F32)
        u2 = k.sb("u2", [128, 8], F32)
        u3 = k.sb("u3", [128, 8], F32)
        u4 = k.sb("u4", [128, 8], F32)
        cre = k.sb("cre", [128, 16], F32)
        cim = k.sb("cim", [128, 16], F32)
        ncim = k.sb("ncim", [128, 16], F32)
        k.memset(cre[:, :], 0.0)
        k.memset(cim[:, :], 0.0)
        k.memset(ncim[:, :], 0.0)

        def genA(c):
            n = c // 4
            cs = slice(c * 128, (c + 1) * 128)
            t1, t2 = T1[c % 2], T2[c % 2]
            for beta in range(8):
                pv = psb[beta % 2]
                kap, eta = beta // 2, beta % 2
                rs = slice(64 * eta, 64 * eta + 64)
                k.mm(pv[:, :], V(uT_t[n], uT.t[rs, kap, cs]), Bbd[rs, kap, :])
                yield
                ws = slice(beta * 512, (beta + 1) * 512)
                k.tt(t1[:, ws], pv[:, :], WA[:, ws], ALU.mult)
                yield
                p4 = pv.t[:, :].rearrange("p (a r c) -> p a r c", r=2, c=128)
                w4 = WB.t[:, ws].rearrange("p (a r c) -> p a r c", r=2, c=128)
                o4 = t2.t[:, ws].rearrange("p (a r c) -> p a r c", r=2, c=128)
                k.tt(V(t2, o4[:, :, 0, :]), V(pv, p4[:, :, 1, :]), V(WB, w4[:, :, 0, :]), ALU.mult)
                yield
                k.tt(V(t2, o4[:, :, 1, :]), V(pv, p4[:, :, 0, :]), V(WB, w4[:, :, 1, :]), ALU.mult)
                yield

        def genB(c):
            n = c // 4
            cs = slice(c * 128, (c + 1) * 128)
            t1, t2 = T1[c % 2], T2[c % 2]
            for hf in range(2):
                pzr = [psb[2], psb[3]]
                pzi = [psb[4], psb[5]]
                for pp in range(8):
                    pair = hf * 8 + pp
                    base = pair * 256
                    orr = pzr[pp // 4][:, (pp % 4) * 128:(pp % 4 + 1) * 128]
                    oii = pzi[pp // 4][:, (pp % 4) * 128:(pp % 4 + 1) * 128]
                    k.mm(orr, t1[:, base:base + 128], tri_b, start=True, stop=False, inc=False)
                    k.mm(orr, t2[:, base:base + 128], tri_b, start=False, stop=True, inc=(pp % 4 == 3))
                    k.mm(oii, t1[:, base + 128:base + 256], tri_b, start=True, stop=False, inc=False)
                    k.mm(oii, t2[:, base + 128:base + 256], tri_b, start=False, stop=True, inc=(pp % 4 == 3))
                    yield
                ps8 = slice(hf * 8, hf * 8 + 8)
                for i in range(2):
                    fs = slice(i * 512, (i + 1) * 512)
                    cb_r = V(cre, cre.t[:, hf * 8 + i * 4:hf * 8 + i * 4 + 4].unsqueeze(2).to_broadcast([128, 4, 128]))
                    cb_i = V(cim, cim.t[:, hf * 8 + i * 4:hf * 8 + i * 4 + 4].unsqueeze(2).to_broadcast([128, 4, 128]))
                    zr3 = V(zr, zr.t[:, fs].rearrange("p (a t) -> p a t", t=128))
                    zi3 = V(zin, zin.t[:, fs].rearrange("p (a t) -> p a t", t=128))
                    pr3 = V(pzr[i], pzr[i].t[:, :].rearrange("p (a t) -> p a t", t=128))
                    pi3 = V(pzi[i], pzi[i].t[:, :].rearrange("p (a t) -> p a t", t=128))
                    k.tt(zr3, pr3, cb_r, ALU.add)
                    yield
                    k.stt(zi3, pi3, -1.0, cb_i, ALU.mult, ALU.subtract)
                    yield
                for i in range(2):
                    lr = V(pzr[i], pzr[i].t[:, :].rearrange("p (a t) -> p a t", t=128)[:, :, 127])
                    li = V(pzi[i], pzi[i].t[:, :].rearrange("p (a t) -> p a t", t=128)[:, :, 127])
                    o_r, c_r = czr[:, i * 4:(i + 1) * 4], cre[:, hf * 8 + i * 4:hf * 8 + i * 4 + 4]
                    k.op('dve', lambda e, o_r=o_r, lr=lr, c_r=c_r: e.tensor_tensor(out=o_r.ap, in0=lr.ap, in1=c_r.ap, op=ALU.add),
                         [lr, c_r, zr[:, :], zin[:, :]], [o_r])
                    yield
                    o_i, c_i = czi[:, i * 4:(i + 1) * 4], cim[:, hf * 8 + i * 4:hf * 8 + i * 4 + 4]
                    k.op('dve', lambda e, o_i=o_i, li=li, c_i=c_i: e.tensor_tensor(out=o_i.ap, in0=li.ap, in1=c_i.ap, op=ALU.add),
                         [li, c_i, zr[:, :], zin[:, :]], [o_i])
                    yield
                PreH = V(Preb, Preb.t[:, ps8, :].rearrange("p a t -> p (a t)"))
                PimH = V(Pimb, Pimb.t[:, ps8, :].rearrange("p a t -> p (a t)"))
                nPimH = V(nPimb, nPimb.t[:, ps8, :].rearrange("p a t -> p (a t)"))
                fl_ = lambda b_: V(b_, b_.t[:, :, :].rearrange("p a t -> p (a t)"))
                k.tt(fl_(Q1[hf]), zr[:, :], PreH, ALU.mult)
                yield
                k.tt(fl_(Q3[hf]), zr[:, :], nPimH, ALU.mult)
                yield
                k.tt(fl_(Q2[hf]), zin[:, :], PimH, ALU.mult)
                yield
                k.tt(fl_(Q4[hf]), zin[:, :], PreH, ALU.mult)
                yield
                lPre = V(Pre, Pre.t[:, ps8, 127])
                lPim = V(Pim, Pim.t[:, ps8, 127])
                k.tt(u1[:, :], czr[:, :], lPre, ALU.mult)
                yield
                k.tt(u2[:, :], czi[:, :], lPim, ALU.mult)
                yield
                k.tt(u3[:, :], czr[:, :], lPim, ALU.mult)
                yield
                k.tt(u4[:, :], czi[:, :], lPre, ALU.mult)
                yield
                k.tt(cre[:, ps8], u1[:, :], u2[:, :], ALU.subtract)
                yield
                k.tt(cim[:, ps8], u3[:, :], u4[:, :], ALU.add)
                yield
                k.ts(ncim[:, ps8], cim[:, ps8], -1.0, ALU.mult)
                yield
            py = psb[6 + c % 2]
            for kap in range(4):
                for pp in range(4):
                    pair = kap * 4 + pp
                    hf, p8 = pair // 8, pair % 8
                    o_ = py[:, kap * 128:(kap + 1) * 128]
                    k.mm(o_, Cbd[:, pair, 0, :], Q1[hf][:, p8, :], start=(pp == 0), stop=False, inc=False)
                    k.mm(o_, Cbd[:, pair, 0, :], Q2[hf][:, p8, :], start=False, stop=False, inc=False)
                    k.mm(o_, Cbd[:, pair, 1, :], Q3[hf][:, p8, :], start=False, stop=False, inc=False)
                    k.mm(o_, Cbd[:, pair, 1, :], Q4[hf][:, p8, :], start=False, stop=(pp == 3), inc=(pp == 3))
                yield
            for kap in range(4):
                k.stt(V(yss_c[c], yss.t[:, kap, cs]), V(uT_t[n], uT.t[:, kap, cs]), col(l, "ssm_D", kap),
                      py[:, kap * 128:(kap + 1) * 128], ALU.mult, ALU.add)
                yield

        for _ in genA(0):
            pass
        for c in range(NCH):
            gb = genB(c)
            ga = genA(c + 1) if c + 1 < NCH else iter(())
            alive = True
            while alive:
                alive = False
                for g_ in (gb, ga):
                    try:
                        next(g_)
                        alive = True
                    except StopIteration:
                        pass
        k.pop()
        k.pop()
        if stop_after == "ssmloop":
            k.push()
            tmpd = k.sb("tmpd", [128, T], F32)
            d_ = di["dbg_yssm"]
            for m in range(4):
                k.op('dve', lambda e, m=m: e.tensor_copy(out=tmpd.t[:, :], in_=yss.t[:, m, :]), [V(b_, yss.t) for b_ in yss_c], [tmpd[:, :]])
                k.dma('sp', V(d_, d_.t[m * 128:(m + 1) * 128, :]), tmpd[:, :])
            k.pop()
            k.pop()
            return
        k.push()
        wglu = k.sb("wglu", [128, 4, 512], BF16)
        k.dma('pool', wglu[:, :, :], V(w_glu, w_glu.t[l].rearrange("(c p) n -> p c n", p=128)))
        wo = k.sb("wo", [128, 4, D], BF16)
        k.dma('pool', wo[:, :, :], V(w_out, w_out.t[l, 0:512, :].rearrange("(c p) n -> p c n", p=128)))
        yg = [k.sb("yg", [128, 4, TS], BF16) for _ in range(2)]
        yf = [k.sb("yf", [128, 4, TS], F32) for _ in range(2)]
        e1_2 = [k.sb("e1", [128, TS], F32) for _ in range(2)]
        e2_2 = [k.sb("e2", [128, TS], F32) for _ in range(2)]
        sg_2 = [k.sb("sg", [128, TS], F32) for _ in range(2)]
        sq = [[k.sb("sq2", [128, TS], BF16) for _ in range(2)] for _ in range(2)]
        rs2 = [k.sb("rs_", [128, TS], F32) for _ in range(2)]
        mx = [k.sb("mx", [128, 4, TS], BF16) for _ in range(2)]
        hb = [k.sb("hb2", [128, 8, TS], F32) for _ in range(2)]

        def glu_body(n):
            par = n % 2
            bset = psb[3 * par:3 * par + 3]
            alt = Banks(bset[1:3])
            e1, e2, sg, rs_ = e1_2[par], e2_2[par], sg_2[par], rs2[par]
            sl = slice(n * TS, (n + 1) * TS)
            ygn, yfn, mxn, hbn = yg[par], yf[par], mx[par], hb[par]
            k.dma('sp', hbn[:, :, :], hview(Hin, n))
            yield
            for c4 in range(4):
                yv = V(yss_c[n * 4], yss.t[:, c4, sl])
                ins_extra = [V(yss_c[n * 4 + i], yss.t[:, c4, sl]) for i in range(1, 4)]
                k.op('dve', lambda e, yv=yv: e.tensor_tensor(out=e1.t[:, :], in0=yv.ap, in1=yv.ap, op=ALU.mult), [yv] + ins_extra, [e1[:, :]])
                k.ts(e1[:, :], e1[:, :], 0.044715, ALU.mult, 1.0, ALU.add)
                yield
                k.tt(e1[:, :], e1[:, :], yv, ALU.mult)
                k.act(e2[:, :], e1[:, :], AF.Sigmoid, scale=1.5957691216057308)
                yield
                k.tt(yfn[:, c4, :], e2[:, :], yv, ALU.mult)
                k.copy(ygn[:, c4, :], yfn[:, c4, :], eng='act')
                yield
            pss = bset[0]
            for m in range(4):
                pg = alt.next()
                for c4 in range(4):
                    k.mm(pg[:, :], wglu[:, c4, m * 128:(m + 1) * 128], ygn[:, c4, :], start=(c4 == 0), stop=(c4 == 3))
                k.act(sg[:, :], pg[:, :], AF.Sigmoid, bias=col(l, "b_glu", m))
                yield
                k.tt(yfn[:, m, :], yfn[:, m, :], sg[:, :], ALU.mult)
                k.act(sq[par][m % 2][:, :], yfn[:, m, :], AF.Square)
                k.mm(pss[:, :], ones_b, sq[par][m % 2][:, :], start=(m == 0), stop=(m == 3), inc=True)
                yield
            rstd_from_ps(pss[:, :], rs_[:, :], 512)
            yield
            for m in range(4):
                k.stt(mxn[:, m, :], yfn[:, m, :], col(l, "ssm_out_norm", m), rs_[:, :], ALU.mult, ALU.mult)
            yield
            if "yssm" in dbg and l == dbg.get("_layer", 0):
                for m in range(4):
                    k.dma('sp', V(di["dbg_yssm"], di["dbg_yssm"].t[m * 128:(m + 1) * 128, sl]), yfn[:, m, :])
            for m in range(8):
                po = alt.next()
                for c4 in range(4):
                    k.mm(po[:, :], wo[:, c4, m * 128:(m + 1) * 128], mxn[:, c4, :], start=(c4 == 0), stop=(c4 == 3))
                k.tt(hbn[:, m, :], hbn[:, m, :], po[:, :], ALU.add)
                yield
            k.dma('sp', hview(Hmid, n), hbn[:, :, :])
            yield

        interleave([(lambda n=n: glu_body(n)) for n in range(NT)], depth=COOP_DEPTH)
        k.pop()
        k.pop()

    def mla_phase(l, qn, qn_t, kvn, kvn_t, krope, kr_t):
        k.push()
        rC = k.sb("rC", [96, T], F32)
        rS = k.sb("rS", [96, T], F32)
        k.dma('sp', rC[:, :], di["ropeC"][:, :])
        k.dma('sp', rS[:, :], di["ropeS"][:, :])
        wq = k.sb("wq", [128, 2, 768], BF16)
        k.dma('pool', wq[:, :, :], V(w_q_up, w_q_up.t[l].rearrange("(c p) n -> p c n", p=128)))
        wkv = k.sb("wkv", [128, 1024], BF16)
        k.dma('pool', wkv[:, :], V(w_kv_up, w_kv_up.t[l]))
        Vt = k.sb("Vt", [128, NCH, 8, 65], BF16)
        k.memset(V(Vt, Vt.t[:, :, :, 64:65]), 1.0)
        wkv_v = V(wkv, wkv.t[:, :].rearrange("p (h x) -> p h x", x=128)[:, :, 64:128])
        for c in range(NCH):
            n = c // 4
            pv = nps()
            k.mm(V(pv, pv.t[:, :].rearrange("p (h x) -> p h x", x=64)), V(kvn_t[n], kvn.t[:, c * 128:(c + 1) * 128]), wkv_v)
            k.copy(V(Vt, Vt.t[:, c, :, 0:64]), V(pv, pv.t[:, :].rearrange("p (h x) -> p h x", x=64)), eng=('act' if c % 2 else 'dve'))
        QT = [k.sb("QT", [96, T], BF16) for _ in range(2)]
        KT = [k.sb("KT", [96, T], BF16) for _ in range(2)]
        kpre = k.sb("kpre", [96, TS], F32)
        sqh = k.sb("sqh", [96, TS], BF16)
        rh = k.sb("rh", [96, TS], F32)
        xg = k.sb("xg", [96, TS], F32)
        xgb = k.sb("xgb", [96, TS], BF16)
        xr1 = k.sb("xr1", [96, TS], F32)
        xr2 = k.sb("xr2", [96, TS], F32)
        PT = [k.sb("PT", [128, TS], BF16) for _ in range(4)]
        rsum = k.sb("rsum", [128, TS], F32)
        rbc = k.sb("rbc", [64, TS], F32)
        osb = [k.sb("osb", [64, TS], F32) for _ in range(2)]
        ones96 = cstb[0:96, 512:608]
        ones_r64 = cstb[64:65, 512:576]
        O_h = O_state["O_h"]
        O_t = [[O_h[h]] * NT for h in range(8)]
        rot2 = [0]

        def nps2():
            rot2[0] += 1
            return psb[3 + rot2[0] % 3]

        BQ = dict(sqh=k.sb("sqhq", [96, TS], BF16), rh=k.sb("rhq", [96, TS], F32), xg=k.sb("xgq", [96, TS], F32),
                  xgb=k.sb("xgbq", [96, TS], BF16), xr1=k.sb("xr1q", [96, TS], F32), xr2=k.sb("xr2q", [96, TS], F32))
        BK = dict(sqh=sqh, rh=rh, xg=xg, xgb=xgb, xr1=xr1, xr2=xr2, pre=kpre, bank=psb[4])
        BQ["pre"] = k.sb("qpre", [96, TS], F32)
        BQ["bank"] = psb[3]

        def chain(h, n, which, B):
            Q, Kh = QT[h % 2], KT[h % 2]
            sqh_, rh_, xg_, xgb_, xr1_, xr2_ = B["sqh"], B["rh"], B["xg"], B["xgb"], B["xr1"], B["xr2"]
            sl = slice(n * TS, (n + 1) * TS)
            pre = B["pre"]
            pp = B["bank"]
            if which == 0:
                for c2 in range(2):
                    k.mm(pp[0:96, :], wq[:, c2, h * 96:(h + 1) * 96], V(qn_t[n], qn.t[:, c2, sl]), start=(c2 == 0), stop=(c2 == 1))
                yield
                k.copy(pre[:, :], pp[0:96, :], eng='dve')
                yield
                gain = col(l, "q_gain", 0, 96)
                dst = Q
            else:
                k.mm(pp[0:64, :], wkv[:, h * 128:h * 128 + 64], V(kvn_t[n], kvn.t[:, sl]))
                yield
                k.copy(pre[0:64, :], pp[0:64, :], eng='dve')
                yield
                k.copy(pre[64:96, :], V(kr_t[n], krope.t[64:96, sl]), eng='dve')
                yield
                gain = col(l, "k_gain", 0, 96)
                dst = Kh
            src = pre[:, :]
            k.tt(sqh_[:, :], src, src, ALU.mult)
            yield
            pss = pp
            k.mm(pss[0:96, :], ones96, sqh_[:, :])
            yield
            k.ts(rh_[:, :], pss[0:96, :], 1.0 / 96, ALU.mult, EPS, ALU.add)
            yield
            k.act(rh_[:, :], rh_[:, :], AF.Ln)
            yield
            k.act(rh_[:, :], rh_[:, :], AF.Exp, scale=-0.5)
            yield
            k.stt(xg_[:, :], src, gain, rh_[:, :], ALU.mult, ALU.mult)
            yield
            k.copy(xgb_[:, :], xg_[:, :], eng='dve')
            yield
            psw = pp
            k.mm(psw[0:96, :], swapT_b, xgb_[:, :])
            yield
            k.tt(xr1_[:, :], xg_[:, :], rC[:, sl], ALU.mult)
            yield
            k.tt(xr2_[:, :], psw[0:96, :], rS[:, sl], ALU.mult)
            yield
            k.tt(dst[:, sl], xr1_[:, :], xr2_[:, :], ALU.add)
            yield

        def prep_gen(h):
            for n in range(NT):
                gens = [chain(h, n, 0, BQ), chain(h, n, 1, BK)]
                while gens:
                    for g_ in list(gens):
                        try:
                            next(g_)
                            yield
                        except StopIteration:
                            gens.remove(g_)

        for _ in prep_gen(0):
            pass
        LOOK = 2
        SC = float(96 ** -0.5)
        for h in range(8):
            Q, Kh = QT[h % 2], KT[h % 2]
            gen = prep_gen(h + 1) if h < 7 else iter(())
            iters = [(sbq, kb) for sbq in range(NT) for kb in range(4 * sbq + 4)]
            Sinfo = {}

            def emit_S(i):
                sbq, kb = iters[i]
                r = kb - 4 * sbq
                q0 = sbq * TS + (max(r, 0)) * 128
                q1_ = (sbq + 1) * TS
                w = q1_ - q0
                pS = psb[i % 3]
                k.mm(pS[:, 0:w], Kh[:, kb * 128:(kb + 1) * 128], Q[:, q0:q1_])
                Sinfo[i] = (pS, w, r, q0 - sbq * TS)

            for i in range(min(LOOK, len(iters))):
                emit_S(i)
            for i, (sbq, kb) in enumerate(iters):
                if i + LOOK < len(iters):
                    emit_S(i + LOOK)
                pS, w, r, off = Sinfo.pop(i)
                nkb = 4 * sbq + 4
                po = psb[6 + sbq % 2]
                pt = PT[i % 4]
                if r >= 0:
                    k.act(pt[:, 0:64], pS[:, 0:64], AF.Exp, scale=SC, bias=cst[:, 480:481])
                    k.act(pt[:, 64:w], pS[:, 64:w], AF.Exp, scale=SC)
                else:
                    k.act(pt[:, 0:w], pS[:, 0:w], AF.Exp, scale=SC)
                k.mm(po[0:65, off:TS], Vt[:, kb, h, :], pt[:, 0:w], start=(kb == 0), stop=(kb == nkb - 1), inc=True)
                if kb == nkb - 1:
                    k.act(rsum[64:65, :], po[64:65, :], AF.Ln)
                    k.act(rsum[64:65, :], rsum[64:65, :], AF.Exp, scale=-1.0)
                    pb = psb[5]
                    k.mm(pb[0:64, :], cst[64:65, 512:576], rsum[64:65, :])
                    k.copy(rbc[:, :], pb[0:64, :], eng='dve')
                    ob = osb[sbq % 2]
                    k.tt(ob[:, :], po[0:64, :], rbc[:, :], ALU.mult)
                    k.dma('sp', V(O_t[h][sbq], oscr.t[h, :, sbq * TS:(sbq + 1) * TS]), ob[:, :])
                next(gen, None)
                next(gen, None)
            for _ in gen:
                pass
        k.pop()

    def mla_out(l, Hin, Hmid):
        O_h = O_state["O_h"]
        O_t = [[O_h[h]] * NT for h in range(8)]
        k.push()
        wo = k.sb("wo2", [64, 8, D], BF16)
        k.dma('pool', wo[:, :, :], V(w_out, w_out.t[l, 512:1024, :].rearrange("(h p) n -> p h n", p=64)))
        ot = [k.sb("ot", [64, 8, TS], F32) for _ in range(2)]
        otl = [[Buf(f"otl{i}_{h}", ot[i].t) for h in range(8)] for i in range(2)]
        sq = [[k.sb("sq3", [64, TS], BF16) for _ in range(2)] for _ in range(2)]
        rs2 = [k.sb("rs3", [64, TS], F32) for _ in range(2)]
        mx = [k.sb("mx3", [64, 8, TS], BF16) for _ in range(2)]
        hb = [k.sb("hb3", [128, 8, TS], F32) for _ in range(2)]
        ones64 = cstb[0:64, 512:576]

        def mo_body(n):
            par = n % 2
            bk = Banks(psb[3 * par:3 * par + 3])
            rs_ = rs2[par]
            sl = slice(n * TS, (n + 1) * TS)
            otn, mxn, hbn = ot[par], mx[par], hb[par]
            k.dma('sp', hbn[:, :, :], hview(Hin, n))
            yield
            pss = bk.next()
            for h in range(8):
                ov = V(otl[par][h], otn.t[:, h, :])
                k.dma('sp', ov, V(O_t[h][n], oscr.t[h, :, sl]))
                k.act(sq[par][h % 2][:, :], ov, AF.Square)
                k.mm(pss[0:64, :], ones64, sq[par][h % 2][:, :], start=(h == 0), stop=(h == 7), inc=True)
                if h % 2 == 1:
                    yield
            rstd_from_ps(pss[0:64, :], rs_[:, :], 512)
            yield
            for h in range(8):
                ov = V(otl[par][h], otn.t[:, h, :])
                k.stt(mxn[:, h, :], ov, col(l, "mla_out_norm", h, 64), rs_[:, :], ALU.mult, ALU.mult)
                if h % 4 == 3:
                    yield
            if "ymla" in dbg and l == dbg.get("_layer", 0):
                for h in range(8):
                    k.dma('sp', V(di["dbg_ymla"], di["dbg_ymla"].t[h * 64:(h + 1) * 64, sl]), V(otl[par][h], otn.t[:, h, :]))
            for m in range(8):
                po = bk.next()
                for h in range(8):
                    k.mm(po[:, :], wo[:, h, m * 128:(m + 1) * 128], mxn[:, h, :], start=(h == 0), stop=(h == 7))
                k.tt(hbn[:, m, :], hbn[:, m, :], po[:, :], ALU.add)
                yield
            k.dma('sp', hview(Hmid, n), hbn[:, :, :])
            yield

        interleave([(lambda n=n: mo_body(n)) for n in range(NT)], depth=COOP_DEPTH)
        k.pop()

    def ffn_layer(l, Hmid, Hout, moe):
        k.push()
        F = 3584 if moe else 2816
        NE = 8 if moe else 1
        GS = 4
        nfc = F // 128
        groups = [(g0, min(GS, nfc - g0)) for g0 in range(0, nfc, GS)]
        HT = 2048
        gates_tm = k.sb("gates", [128, NCH, 8], F32) if moe else None
        if moe:
            wr = k.sb("wr", [128, 8, 8], F32)
            k.dma('sp', wr[:, :, :], V(mwr, mwr.t[0].rearrange("(c p) e -> p c e", p=128)))
        for half in range(2):
            k.push()
            zT = k.sb("zT", [128, 8, HT], BF16)
            acc = k.sb("acc", [128, 8, HT], F32)
            zT_t = [Buf(f"zT{i}", zT.t) for i in range(4)]
            acc_t = [[Buf(f"acc{i}_{m}", acc.t) for m in range(8)] for i in range(4)]
            k.push()
            hbuf = [k.sb("hbuf", [128, 8, TS], F32) for _ in range(2)]
            sq = [[k.sb("sq", [128, TS], BF16) for _ in range(2)] for _ in range(2)]
            rstd2 = [k.sb("rstd", [128, TS], F32) for _ in range(2)]
            zf2 = [k.sb("zf", [128, 8, TS], F32) for _ in range(2)] if moe else [None, None]
            rt = [dict(lg=k.sb("lg", [128, 8], F32), mx8=k.sb("mx8", [128, 8], F32), msk=k.sb("msk", [128, 8], F32),
                       ee=k.sb("ee", [128, 8], F32), nm1=k.sb("nm1", [128, 1], F32), ssum=k.sb("ssum", [128, 1], F32))
                  for _ in range(2)]

            def fn_body(i):
                par = i % 2
                bk = Banks(psb[3 * par:3 * par + 3])
                rstd, zf = rstd2[par], zf2[par]
                lg, mx8, msk, ee, nm1, ssum = (rt[par][x] for x in ("lg", "mx8", "msk", "ee", "nm1", "ssum"))
                n = half * 4 + i
                hb = hbuf[par]
                k.dma('sp', hb[:, :, :], hview(Hmid, n))
                yield
                pss = bk.next()
                for c in range(8):
                    k.act(sq[par][c % 2][:, :], hb[:, c, :], AF.Square)
                    k.mm(pss[:, :], ones_b, sq[par][c % 2][:, :], start=(c == 0), stop=(c == 7), inc=True)
                    if c % 2 == 1:
                        yield
                rstd_from_ps(pss[:, :], rstd[:, :], D)
                yield
                for c in range(8):
                    dstv = V(zT_t[i], zT.t[:, c, i * TS:(i + 1) * TS])
                    if moe:
                        k.stt(zf[:, c, :], hb[:, c, :], col(l, "ffn_norm", c), rstd[:, :], ALU.mult, ALU.mult)
                        k.copy(dstv, zf[:, c, :], eng='act')
                    else:
                        k.stt(dstv, hb[:, c, :], col(l, "ffn_norm", c), rstd[:, :], ALU.mult, ALU.mult)
                    k.copy(V(acc_t[i][c], acc.t[:, c, i * TS:(i + 1) * TS]), hb[:, c, :], eng='act')
                    if c % 2 == 1:
                        yield
                if moe:
                    for b4 in range(4):
                        blk = n * 4 + b4
                        pl = bk.next()
                        for c in range(8):
                            k.mm(pl[:, 0:8], zf[:, c, b4 * 128:(b4 + 1) * 128], wr[:, c, :], start=(c == 0), stop=(c == 7))
                        k.copy(lg[:, :], pl[:, 0:8])
                        yield
                        k.op('dve', lambda e: e.max(out=mx8.t[:, :], in_=lg.t[:, :]), [lg[:, :]], [mx8[:, :]])
                        k.ts(msk[:, :], lg[:, :], mx8[:, 1:2], ALU.is_ge)
                        k.ts(nm1[:, :], mx8[:, 0:1], -1.0, ALU.mult)
                        yield
                        k.act(ee[:, :], lg[:, :], AF.Exp, bias=nm1[:, 0:1])
                        k.tt(ee[:, :], ee[:, :], msk[:, :], ALU.mult)
                        yield
                        k.op('dve', lambda e: e.reduce_sum(out=ssum.t[:, :], in_=ee.t[:, :], axis=AX.X), [ee[:, :]], [ssum[:, :]])
                        k.op('dve', lambda e: e.reciprocal(out=ssum.t[:, :], in_=ssum.t[:, :]), [ssum[:, :]], [ssum[:, :]])
                        k.ts(V(gates_tm, gates_tm.t[:, blk, :]), ee[:, :], ssum[:, 0:1], ALU.mult)
                        yield

            interleave([(lambda i=i: fn_body(i)) for i in range(4)], depth=COOP_DEPTH)
            k.pop()
            k.push()
            wg = [k.sb("wg", [128, 8, GS * 128], BF16) for _ in range(2)]
            wu = [k.sb("wu", [128, 8, GS * 128], BF16) for _ in range(2)]
            wd = [k.sb("wd", [128, GS, D], BF16) for _ in range(2)]
            aT = [k.sb("aT", [128, GS, HT], BF16) for _ in range(2)]
            sg = [k.sb("sg", [128, TS], F32) for _ in range(2)]
            tg = [k.sb("tg", [128, TS], F32) for _ in range(2)]
            G = k.sb("G", [128, HT], F32) if moe else None
            gi = 0
            for e in range(NE):
                if moe:
                    Wg, Wu, Wd = mwg.t[0, e], mwu.t[0, e], mwd.t[0, e]
                    WgB, WuB, WdB = mwg, mwu, mwd
                    for b16 in range(16):
                        blk = half * 16 + b16
                        pgx = nps()
                        k.mm(pgx[:, 0:128], V(gates_tm, gates_tm.t[:, blk, e:e + 1].to_broadcast([128, 128])), ident_f)
                        k.copy(G[:, b16 * 128:(b16 + 1) * 128], pgx[:, 0:128], eng='act')
                else:
                    Wg, Wu, Wd = dwg.t[0], dwu.t[0], dwd.t[0]
                    WgB, WuB, WdB = dwg, dwu, dwd
                for (g0, gn) in groups:
                    b = gi % 2
                    gi += 1
                    fs = slice(g0 * 128, (g0 + gn) * 128)
                    k.dma('pool', V(wg[b], wg[b].t[:, :, 0:gn * 128]), V(WgB, Wg[:, fs].rearrange("(c p) f -> p c f", p=128)))
                    k.dma('pool', V(wu[b], wu[b].t[:, :, 0:gn * 128]), V(WuB, Wu[:, fs].rearrange("(c p) f -> p c f", p=128)))
                    k.dma('pool', V(wd[b], wd[b].t[:, 0:gn, :]), V(WdB, Wd[fs, :].rearrange("(c p) n -> p c n", p=128)))
                    a = aT[b]
                    for fc in range(gn):
                        for i in range(4):
                            tsl = slice(i * TS, (i + 1) * TS)
                            pg_, pu_ = nps(), nps()
                            for c in range(8):
                                k.mm(pg_[:, :], wg[b][:, c, fc * 128:(fc + 1) * 128], V(zT_t[i], zT.t[:, c, tsl]), start=(c == 0), stop=(c == 7))
                            for c in range(8):
                                k.mm(pu_[:, :], wu[b][:, c, fc * 128:(fc + 1) * 128], V(zT_t[i], zT.t[:, c, tsl]), start=(c == 0), stop=(c == 7))
                            s_ = sg[(fc * 4 + i) % 2]
                            t_ = tg[(fc * 4 + i) % 2]
                            k.act(s_[:, :], pg_[:, :], AF.Silu)
                            if moe:
                                k.tt(t_[:, :], pu_[:, :], s_[:, :], ALU.mult)
                                k.tt(a[:, fc, tsl], t_[:, :], G[:, tsl], ALU.mult, eng='pool')
                            else:
                                k.tt(a[:, fc, tsl], pu_[:, :], s_[:, :], ALU.mult)
                    for i in range(4):
                        tsl = slice(i * TS, (i + 1) * TS)
                        for m in range(8):
                            pd = nps()
                            for fc in range(gn):
                                k.mm(pd[:, :], wd[b][:, fc, m * 128:(m + 1) * 128], a[:, fc, tsl], start=(fc == 0), stop=(fc == gn - 1))
                            av = V(acc_t[i][m], acc.t[:, m, tsl])
                            k.tt(av, av, pd[:, :], ALU.add)
            for i in range(4):
                n = half * 4 + i
                ins_all = [V(acc_t[i][m], acc.t[:, m, i * TS:(i + 1) * TS]) for m in range(8)]
                src = V(acc_t[i][0], acc.t[:, :, i * TS:(i + 1) * TS])
                k._waits('sp', [x.b for x in ins_all], [])
                k.dma('sp', hview(Hout, n), src)
                ev = Hout[n].w
                for x in ins_all:
                    x.b.r[ev[0]] = ev[1]
            k.pop()
            k.pop()
        k.pop()

    ok0 = mixer_layer(0, H_x, H_A)
    if stop_after in ("p1", "mla", "mlaout", "ssmtab", "ssmloop"):
        final = H_A if ok0 else H_x
    elif stop_after == "mix0":
        final = H_A
    else:
        ffn_layer(0, H_A, H_B, moe=False)
        if stop_after == "ffn0":
            final = H_B
        else:
            mixer_layer(1, H_B, H_A)
            if stop_after == "mix1":
                final = H_A
            else:
                ffn_layer(1, H_A, H_O, moe=True)
                final = H_O
    if final is not H_O:
        k.push()
        tb = [k.sb("tb", [128, 8, TS], F32) for _ in range(2)]
        for n in range(NT):
            k.dma('sp', tb[n % 2][:, :, :], hview(final, n))
            k.dma('sp', hview(H_O, n), tb[n % 2][:, :, :])
        k.pop()
    k.barrier()
    k.pop()
    k.es.close()
    return nc


def host_prep(inputs):
    f = np.float32
    shared = {}
    for nm in ["w_in", "ssm_w_glu", "mla_w_q_up", "mla_w_kv_up", "w_out", "dense_w_gate", "dense_w_up",
               "dense_w_down", "moe_w_router", "moe_w_gate", "moe_w_up", "moe_w_down"]:
        shared[nm] = np.ascontiguousarray(inputs[nm], dtype=f)
    cols = np.zeros((128, NCOLS), f)

    def put(l, name, arr2d):
        o, w = CP[name]
        cols[:arr2d.shape[0], l * CPL + o:l * CPL + o + w] = arr2d
    for l in range(2):
        put(l, "attn_norm", inputs["attn_norm"][l].reshape(8, 128).T)
        put(l, "ffn_norm", inputs["ffn_norm"][l].reshape(8, 128).T)
        put(l, "q_norm", inputs["mla_q_norm"][l].reshape(2, 128).T)
        put(l, "kv_norm", inputs["mla_kv_norm"][l].reshape(1, 128).T)
        put(l, "q_gain", inputs["mla_q_gain"][l].reshape(1, 96).T)
        put(l, "k_gain", inputs["mla_k_gain"][l].reshape(1, 96).T)
        put(l, "ssm_out_norm", inputs["ssm_out_norm"][l].reshape(4, 128).T)
        put(l, "mla_out_norm", inputs["mla_out_norm"][l].reshape(8, 64).T)
        put(l, "b_glu", inputs["ssm_b_glu"][l].reshape(4, 128).T)
        put(l, "ssm_D", inputs["ssm_D"][l].reshape(4, 128).T)
        put(l, "fA_re", inputs["ssm_A_re"][l].reshape(16, 2, 64).transpose(1, 2, 0).reshape(128, 16))
        put(l, "fA_im", inputs["ssm_A_im"][l].reshape(16, 2, 64).transpose(1, 2, 0).reshape(128, 16))
        put(l, "fLs", np.broadcast_to(inputs["ssm_log_step"][l].reshape(16, 2, 1), (16, 2, 64)).transpose(1, 2, 0).reshape(128, 16))
    cols[:, C_J1] = np.arange(1, 129, dtype=f)
    shared["cols"] = cols
    def rowtab(a_gp):
        t = a_gp.reshape(16, 1, 2, 64)
        t = np.broadcast_to(t, (16, 2, 2, 64)).reshape(1, 4096)
        return np.ascontiguousarray(np.broadcast_to(t, (128, 4096)), dtype=f)
    shared["rowA_re"] = np.stack([rowtab(inputs["ssm_A_re"][l]) for l in range(2)])
    shared["rowA_im"] = np.stack([rowtab(inputs["ssm_A_im"][l]) for l in range(2)])
    shared["rowLs"] = np.stack([rowtab(np.broadcast_to(inputs["ssm_log_step"][l][:, None], (32, 64))) for l in range(2)])
    def bbtab(a_gp, l):
        t = a_gp.reshape(4, 8, 1, 1, 64)
        t = np.broadcast_to(t, (4, 8, 16, 8, 64))
        t = t.transpose(1, 2, 0, 3, 4).reshape(128, 2048)
        return np.ascontiguousarray(t, dtype=f)
    shared["bbA_re"] = np.stack([bbtab(inputs["ssm_A_re"][l], l) for l in range(2)])
    shared["bbA_im"] = np.stack([bbtab(inputs["ssm_A_im"][l], l) for l in range(2)])
    shared["bbLs"] = np.stack([bbtab(np.broadcast_to(inputs["ssm_log_step"][l][:, None], (32, 64)), l) for l in range(2)])

    def bplace(Bm):
        o = np.zeros((8, 16, 4, 2, 2, 2, 64), f)
        for g in range(32):
            kap, g8 = g // 8, g % 8
            pair2, g2 = (g8 % 4) // 2, g8 % 2
            for r in range(2):
                o[g8, :, kap, pair2, r, g2, :] = Bm[g].T
        return o.reshape(128, 2048)
    shared["BR"] = np.stack([bplace(inputs["ssm_B_re"][l]) for l in range(2)])
    shared["BI"] = np.stack([bplace(inputs["ssm_B_im"][l]) for l in range(2)])

    def cplace(Cr, Ci):
        o = np.zeros((2, 64, 16, 2, 8, 16), f)
        for g in range(32):
            pair, g2 = g // 2, g % 2
            g8 = g % 8
            o[g2, :, pair, 0, g8, :] = Cr[g].T
            o[g2, :, pair, 1, g8, :] = Ci[g].T
        return o.reshape(128, 4096)
    shared["Cbd"] = np.stack([cplace(inputs["ssm_C_re"][l], inputs["ssm_C_im"][l]) for l in range(2)])
    cst = np.zeros((128, 640), f)
    cst[:, 0:128] = np.eye(128, dtype=f)
    cst[:, 128:256] = np.triu(np.ones((128, 128), f))
    cst[:, 256:384] = np.arange(1, 129, dtype=f)[None, :]
    sw = np.zeros((96, 96), f)
    for i in range(16):
        sw[80 + i, 64 + i] = -1.0
        sw[64 + i, 80 + i] = 1.0
    cst[0:96, 384:480] = sw
    cst[:, 512:640] = 1.0
    cst[64:128, 480] = -30000.0
    shared["consts"] = cst
    inv = 1.0 / (10000.0 ** (np.arange(0, 32, 2, dtype=np.float32) / 32))
    ang = np.arange(T, dtype=np.float32)[:, None] * inv[None, :].astype(np.float32)
    cs, sn = np.cos(ang).astype(f).T, np.sin(ang).astype(f).T
    rc = np.ones((96, T), f)
    rs = np.zeros((96, T), f)
    rc[64:80] = cs
    rc[80:96] = cs
    rs[64:80] = sn
    rs[80:96] = sn
    shared["ropeC"] = rc
    shared["ropeS"] = rs
    return shared


def kernel(**inputs):
    inputs = {k_: np.asarray(v) for k_, v in inputs.items()}
    shared = host_prep(inputs)
    nc = build()
    x = inputs["x"]
    in_maps = []
    for b in range(8):
        m = dict(shared)
        m["xT"] = np.ascontiguousarray(x[b].T, dtype=np.float32)
        in_maps.append(m)
    res = run_bass_kernel_spmd(nc, in_maps, core_ids=list(range(8)))
    out = np.stack([np.ascontiguousarray(res.results[b]["outT"].T) for b in range(8)])
    return out.astype(np.float32)
```

```python
import threading
import numpy as np
from contextlib import ExitStack
import concourse.bass as bass
import concourse.mybir as mybir
from concourse.bass_utils import run_bass_kernel_spmd

F32 = mybir.dt.float32
BF16 = mybir.dt.bfloat16
AF = mybir.ActivationFunctionType
ALU = mybir.AluOpType
AX = mybir.AxisListType

T = 4096
D = 1024
TS = 512
NT = T // TS
COOP_DEPTH = 2
NCH = 32
PI = float(np.pi)
EPS = 1e-6

CP = {}
_o = 0
for _n, _w in [("attn_norm", 8), ("ffn_norm", 8), ("q_norm", 2), ("kv_norm", 1), ("q_gain", 1),
               ("k_gain", 1), ("ssm_out_norm", 4), ("mla_out_norm", 8), ("b_glu", 4), ("ssm_D", 4),
               ("fA_re", 16), ("fA_im", 16), ("fLs", 16)]:
    CP[_n] = (_o, _w)
    _o += _w
CPL = _o
C_J1 = 2 * CPL
NCOLS = C_J1 + 1


class V:
    __slots__ = ("b", "ap")

    def __init__(s, b, ap):
        s.b = b
        s.ap = ap


class Buf:
    def __init__(s, name, t):
        s.name = name
        s.t = t
        s.w = None
        s.r = {}
        s.dsem = None
        s.dcnt = 0

    def __getitem__(s, idx):
        return V(s, s.t[idx])

    def v(s, ap):
        return V(s, ap)


class Coop:
    def __init__(s):
        s.cv = threading.Condition()
        s.turn = None
        s.live = []
        s.err = None
        s.workers = set()

    def hook(s):
        me = threading.get_ident()
        if me not in s.workers:
            return
        with s.cv:
            if len(s.live) < 2:
                return
            idx = s.live.index(me)
            s.turn = s.live[(idx + 1) % len(s.live)]
            s.cv.notify_all()
            while s.turn != me:
                s.cv.wait()

    def run(s, fns, depth=2):
        pending = list(fns)
        done = threading.Event()
        threads = []

        def spawn(fn):
            t = threading.Thread(target=worker, args=(fn,))
            t.start()
            s.live.append(t.ident)
            s.workers.add(t.ident)
            threads.append(t)

        def worker(fn):
            me = threading.get_ident()
            with s.cv:
                while s.turn != me:
                    s.cv.wait()
            try:
                if s.err is None:
                    fn()
            except BaseException as e:
                s.err = e
            with s.cv:
                idx = s.live.index(me)
                s.live.remove(me)
                s.workers.discard(me)
                if pending:
                    spawn(pending.pop(0))
                if s.live:
                    s.turn = s.live[idx % len(s.live)]
                else:
                    s.turn = None
                    done.set()
                s.cv.notify_all()

        with s.cv:
            for _ in range(min(depth, len(pending))):
                spawn(pending.pop(0))
            s.turn = s.live[0]
            s.cv.notify_all()
        done.wait()
        for t in threads:
            t.join()
        if s.err is not None:
            e, s.err = s.err, None
            raise e


def interleave(fns, depth=2):
    pending = list(fns)
    live = []
    while pending or live:
        while pending and len(live) < depth:
            live.append(pending.pop(0)())
        for g in list(live):
            try:
                next(g)
            except StopIteration:
                live.remove(g)


class Banks:
    def __init__(s, banks):
        s.b = list(banks)
        s.i = -1

    def next(s):
        s.i += 1
        return s.b[s.i % len(s.b)]


class K:
    def __init__(s):
        s.nc = bass.Bass("TRN2", target_bir_lowering=False)
        nc = s.nc
        s.es = ExitStack()
        s.E = {'pe': nc.tensor, 'act': nc.scalar, 'dve': nc.vector, 'pool': nc.gpsimd, 'sp': nc.sync}
        s.sem = {e: s.es.enter_context(nc.semaphore("S_" + e)) for e in ('pe', 'act', 'dve', 'pool')}
        s.cnt = {e: 0 for e in s.sem}
        s.known = {e: {} for e in s.E}
        s.pend = {e: ([], []) for e in s.sem}
        s.dmasems = {}
        s.nsem = 4
        s.uid = 0
        s.coop = Coop()
        s.stacks = []
        s.stack_bufs = []
        s.freesems = []

    def push(s):
        st = ExitStack()
        s.stacks.append(st)
        s.stack_bufs.append([])
        return st

    def pop(s):
        s.barrier()
        st = s.stacks.pop()
        for b in s.stack_bufs.pop():
            if b.dsem is not None:
                s.freesems.append((b.dsem, b.dcnt))
                b.dsem = None
        st.close()

    def sb(s, name, shape, dt):
        s.uid += 1
        t = s.stacks[-1].enter_context(s.nc.sbuf_tensor(f"{name}_{s.uid}", list(shape), dt))
        b = Buf(name, t)
        s.stack_bufs[-1].append(b)
        return b

    def ps(s, name, shape, dt=F32):
        s.uid += 1
        t = s.stacks[-1].enter_context(s.nc.psum_tensor(f"{name}_{s.uid}", list(shape), dt))
        return Buf(name, t)

    def dram(s, name, shape, dt, kind):
        t = s.nc.dram_tensor(name, list(shape), dt, kind=kind).ap()
        return Buf(name, t)

    def newsem(s, name):
        s.nsem += 1
        s.uid += 1
        return s.es.enter_context(s.nc.semaphore(f"D_{name}_{s.uid}"))

    def _waits(s, eng, reads, writes):
        ev = {}
        for b in reads:
            if b.w is not None:
                k, v_ = b.w
                if ev.get(k, 0) < v_:
                    ev[k] = v_
        for b in writes:
            if b.w is not None:
                k, v_ = b.w
                if ev.get(k, 0) < v_:
                    ev[k] = v_
            for k, v_ in b.r.items():
                if ev.get(k, 0) < v_:
                    ev[k] = v_
        kn = s.known[eng]
        E = s.E[eng]
        for k, v_ in ev.items():
            if eng == 'pe' and k is s.sem['pe']:
                continue
            if kn.get(k, 0) >= v_:
                continue
            E.wait_ge(k, v_)
            kn[k] = v_

    def _commit(s, ev, reads, writes):
        k, v_ = ev
        for b in reads:
            if b.r.get(k, 0) < v_:
                b.r[k] = v_
        for b in writes:
            b.w = ev
            b.r = {}

    def op(s, eng, fn, ins, outs, inc=True):
        reads = [x.b for x in ins]
        writes = [x.b for x in outs]
        s._waits(eng, reads, writes)
        I = fn(s.E[eng])
        if inc:
            s.cnt[eng] += 1
            I.then_inc(s.sem[eng], 1)
            ev = (s.sem[eng], s.cnt[eng])
            pr, pw = s.pend[eng]
            s._commit(ev, reads + pr, writes + pw)
            s.pend[eng] = ([], [])
        else:
            pr, pw = s.pend[eng]
            pr.extend(reads)
            pw.extend(writes)
        return I

    def dma(s, q, out, in_, **kw):
        reads = [in_.b]
        writes = [out.b]
        d = out.b
        saved = d.w
        if d.w is not None and d.dsem is not None and d.w[0] is d.dsem:
            d.w = None
        s._waits(q, reads, writes)
        d.w = saved
        if d.dsem is None:
            if s.freesems:
                d.dsem, d.dcnt = s.freesems.pop()
            else:
                d.dsem = s.newsem(d.name)
        d.dcnt += 1
        I = s.E[q].dma_start(out=out.ap, in_=in_.ap, **kw)
        I.then_inc(d.dsem, 16)
        ev = (d.dsem, 16 * d.dcnt)
        s.dmasems[d.dsem] = 16 * d.dcnt
        s._commit(ev, reads, writes)

    def barrier(s):
        for e in s.E:
            kn = s.known[e]
            for e2 in s.sem:
                k, v_ = s.sem[e2], s.cnt[e2]
                if v_ > 0 and kn.get(k, 0) < v_:
                    s.E[e].wait_ge(k, v_)
                    kn[k] = v_
            for k, v_ in s.dmasems.items():
                if kn.get(k, 0) < v_:
                    s.E[e].wait_ge(k, v_)
                    kn[k] = v_

    def mm(s, out, lhsT, rhs, start=True, stop=True, inc=None):
        if inc is None:
            inc = stop
        return s.op('pe', lambda e: e.matmul(out.ap, lhsT=lhsT.ap, rhs=rhs.ap, start=start, stop=stop),
                    [lhsT, rhs], [out], inc=inc)

    def act(s, out, in_, func, bias=None, scale=None, eng='act'):
        ins = [in_]
        kw = {}
        if bias is not None:
            if isinstance(bias, V):
                ins.append(bias)
                kw['bias'] = bias.ap
            else:
                kw['bias'] = bias
        if scale is not None:
            if isinstance(scale, V):
                ins.append(scale)
                kw['scale'] = scale.ap
            else:
                kw['scale'] = scale
        return s.op(eng, lambda e: e.activation(out=out.ap, in_=in_.ap, func=func, **kw), ins, [out])

    def tt(s, out, in0, in1, op, eng='dve'):
        return s.op(eng, lambda e: e.tensor_tensor(out=out.ap, in0=in0.ap, in1=in1.ap, op=op), [in0, in1], [out])

    def ts(s, out, in0, s1, op0, s2=None, op1=None, eng='dve'):
        ins = [in0]
        a1 = s1
        if isinstance(s1, V):
            ins.append(s1)
            a1 = s1.ap
        a2 = s2
        if isinstance(s2, V):
            ins.append(s2)
            a2 = s2.ap
        if op1 is None:
            return s.op(eng, lambda e: e.tensor_scalar(out=out.ap, in0=in0.ap, scalar1=a1, scalar2=None, op0=op0), ins, [out])
        return s.op(eng, lambda e: e.tensor_scalar(out=out.ap, in0=in0.ap, scalar1=a1, scalar2=a2, op0=op0, op1=op1), ins, [out])

    def stt(s, out, in0, sc, in1, op0, op1, eng='dve'):
        ins = [in0, in1]
        a = sc
        if isinstance(sc, V):
            ins.append(sc)
            a = sc.ap
        return s.op(eng, lambda e: e.scalar_tensor_tensor(out=out.ap, in0=in0.ap, scalar=a, in1=in1.ap, op0=op0, op1=op1), ins, [out])

    def copy(s, out, in_, eng='dve'):
        if eng == 'act':
            return s.act(out, in_, AF.Copy)
        return s.op(eng, lambda e: e.tensor_copy(out=out.ap, in_=in_.ap), [in_], [out])

    def memset(s, out, val, eng='dve'):
        return s.op(eng, lambda e: e.memset(out.ap, val), [], [out])


def bc(v, shape):
    return V(v.b, v.ap.to_broadcast(list(shape)))


def build(dbg=None, stop_after=None):
    k = K()
    nc = k.nc
    dbg = dbg or {}
    di = {}

    def inp(name, shape):
        di[name] = k.dram(name, shape, F32, "ExternalInput")
        return di[name]

    xT = inp("xT", [D, T])
    w_in = inp("w_in", [2, D, 928])
    w_glu = inp("ssm_w_glu", [2, 512, 512])
    w_q_up = inp("mla_w_q_up", [2, 256, 768])
    w_kv_up = inp("mla_w_kv_up", [2, 128, 1024])
    w_out = inp("w_out", [2, D, D])
    dwg = inp("dense_w_gate", [1, D, 2816])
    dwu = inp("dense_w_up", [1, D, 2816])
    dwd = inp("dense_w_down", [1, 2816, D])
    mwr = inp("moe_w_router", [1, D, 8])
    mwg = inp("moe_w_gate", [1, 8, D, 3584])
    mwu = inp("moe_w_up", [1, 8, D, 3584])
    mwd = inp("moe_w_down", [1, 8, 3584, D])
    cols_d = inp("cols", [128, NCOLS])
    rowA_re = inp("rowA_re", [2, 128, 4096])
    rowA_im = inp("rowA_im", [2, 128, 4096])
    rowLs = inp("rowLs", [2, 128, 4096])
    bbA_re = inp("bbA_re", [2, 128, 2048])
    bbA_im = inp("bbA_im", [2, 128, 2048])
    bbLs = inp("bbLs", [2, 128, 2048])
    BRd = inp("BR", [2, 128, 2048])
    BId = inp("BI", [2, 128, 2048])
    Cbd_d = inp("Cbd", [2, 128, 4096])
    consts_d = inp("consts", [128, 640])
    ropeC_d = inp("ropeC", [96, T])
    ropeS_d = inp("ropeS", [96, T])

    outT = k.dram("outT", [D, T], F32, "ExternalOutput")
    hscr = [k.dram("hA", [D, T], F32, "Internal"), k.dram("hB", [D, T], F32, "Internal")]
    oscr = k.dram("oscr", [8, 64, T], F32, "Internal")
    for nm, shp in dbg.items():
        if not nm.startswith("_"):
            di["dbg_" + nm] = k.dram("dbg_" + nm, shp, F32, "ExternalOutput")

    def htiles(buf):
        return [Buf(f"{buf.name}_t{n}", buf.t) for n in range(NT)]
    H_x = htiles(xT)
    H_A = htiles(hscr[0])
    H_B = htiles(hscr[1])
    H_O = htiles(outT)

    O_state = {"O_h": [Buf(f"o{h}", oscr.t) for h in range(8)]}

    def hview(tl, n):
        return V(tl[n], tl[n].t.rearrange("(c p) t -> p c t", p=128)[:, :, n * TS:(n + 1) * TS])

    k.push()
    cols = k.sb("cols", [128, NCOLS], F32)
    k.dma('sp', cols[:, :], di["cols"][:, :])
    cst = k.sb("cst", [128, 640], F32)
    k.dma('sp', cst[:, :], di["consts"][:, :])
    cstb = k.sb("cstb", [128, 640], BF16)
    k.copy(cstb[:, :], cst[:, :])
    ident_f = cst[:, 0:128]
    tri_b = cstb[:, 128:256]
    t1row = cst[:, 256:384]
    swapT_b = cstb[0:96, 384:480]
    ones_b = cstb[:, 512:640]
    psb = [k.ps(f"ps{i}", [128, 512]) for i in range(8)]
    pctr = [0]

    def nps():
        pctr[0] += 1
        return psb[pctr[0] % 6]

    def col(l, name, j=0, rows=128):
        o, w = CP[name]
        c = l * CPL + o + j
        return cols[0:rows, c:c + 1]

    def dump(name, view_fn):
        if name in dbg:
            view_fn(di["dbg_" + name])

    def rstd_from_ps(ps_v, out_v, n_feat):
        k.ts(out_v, ps_v, 1.0 / n_feat, ALU.mult, EPS, ALU.add)
        k.act(out_v, out_v, AF.Ln)
        k.act(out_v, out_v, AF.Exp, scale=-0.5)

    def sincos(ang, sin_out, cos_out, tmp, ti):
        for shift, outv in ((0.0, sin_out), (0.5 * PI, cos_out)):
            k.ts(tmp, ang, shift, ALU.add, 1.0 / (2 * PI), ALU.mult)
            k.copy(ti, tmp)
            k.copy(tmp, ti)
            k.stt(tmp, tmp, -2 * PI, ang, ALU.mult, ALU.add)
            if shift != 0.0:
                k.ts(tmp, tmp, shift, ALU.add)
            k.ts(outv, tmp, PI, ALU.is_gt, -2 * PI, ALU.mult)
            k.tt(tmp, tmp, outv, ALU.add)
            k.act(outv, tmp, AF.Sin)

    def mixer_layer(l, Hin, Hmid):
        k.push()
        uT = k.sb("uT", [128, 4, T], BF16)
        uT_t = [Buf(f"uT{n}", uT.t) for n in range(NT)]
        k.push()
        qn = k.sb("qn", [128, 2, T], BF16)
        kvn = k.sb("kvn", [128, T], BF16)
        krope = k.sb("krope", [96, T], BF16)
        qn_t = [Buf(f"qn{n}", qn.t) for n in range(NT)]
        kvn_t = [Buf(f"kvn{n}", kvn.t) for n in range(NT)]
        kr_t = [Buf(f"kr{n}", krope.t) for n in range(NT)]

        if dbg.get("_only_tab"):
            k.pop()
            ssm_phase(l, uT, uT_t, Hmid, Hmid)
            k.pop()
            return False
        k.push()
        winb = k.sb("winb", [128, 8, 928], BF16)
        k.dma('pool', winb[:, :, :], V(w_in, w_in.t[l].rearrange("(c p) n -> p c n", p=128)))
        hbuf = [k.sb("hbuf", [128, 8, TS], F32) for _ in range(2)]
        sq = [[k.sb("sq", [128, TS], BF16) for _ in range(2)] for _ in range(2)]
        rstd2 = [k.sb("rstd", [128, TS], F32) for _ in range(2)]
        zb = [k.sb("zb", [128, 8, TS], BF16) for _ in range(2)]
        sqq2 = [[k.sb("sqq", [128, TS], BF16) for _ in range(2)] for _ in range(2)]
        rq2 = [k.sb("rq", [128, TS], F32) for _ in range(2)]

        def p1_body(n):
            par = n % 2
            bk = Banks(psb[3 * par:3 * par + 3])
            sl = slice(n * TS, (n + 1) * TS)
            hb = hbuf[par]
            z = zb[par]
            rstd, rq, sqq = rstd2[par], rq2[par], sqq2[par]
            k.dma('sp', hb[:, :, :], hview(Hin, n))
            yield
            pss = bk.next()
            for c in range(8):
                sqc = sq[par][c % 2]
                k.act(sqc[:, :], hb[:, c, :], AF.Square)
                k.mm(pss[:, :], ones_b, sqc[:, :], start=(c == 0), stop=(c == 7), inc=True)
                if c % 2 == 1:
                    yield
            rstd_from_ps(pss[:, :], rstd[:, :], D)
            yield
            for c in range(8):
                k.stt(z[:, c, :], hb[:, c, :], col(l, "attn_norm", c), rstd[:, :], ALU.mult, ALU.mult)
                if c % 4 == 3:
                    yield
            for m in range(4):
                p_ = bk.next()
                for c in range(8):
                    k.mm(p_[:, :], winb[:, c, m * 128:(m + 1) * 128], z[:, c, :], start=(c == 0), stop=(c == 7))
                if m % 2 == 0:
                    k.copy(V(uT_t[n], uT.t[:, m, sl]), p_[:, :], eng='act')
                else:
                    k.copy(V(uT_t[n], uT.t[:, m, sl]), p_[:, :], eng='dve')
                yield
            pq = [bk.next(), bk.next()]
            for m in range(2):
                for c in range(8):
                    k.mm(pq[m][:, :], winb[:, c, 512 + m * 128:512 + (m + 1) * 128], z[:, c, :], start=(c == 0), stop=(c == 7))
                yield
            pqs = bk.next()
            for m in range(2):
                k.act(sqq[m][:, :], pq[m][:, :], AF.Square)
                k.mm(pqs[:, :], ones_b, sqq[m][:, :], start=(m == 0), stop=(m == 1), inc=True)
            yield
            rstd_from_ps(pqs[:, :], rq[:, :], 256)
            yield
            for m in range(2):
                k.stt(V(qn_t[n], qn.t[:, m, sl]), pq[m][:, :], col(l, "q_norm", m), rq[:, :], ALU.mult, ALU.mult)
            yield
            pk = bk.next()
            for c in range(8):
                k.mm(pk[:, :], winb[:, c, 768:896], z[:, c, :], start=(c == 0), stop=(c == 7))
            yield
            pks = bk.next()
            k.act(sqq[0][:, :], pk[:, :], AF.Square)
            k.mm(pks[:, :], ones_b, sqq[0][:, :], start=True, stop=True)
            yield
            rstd_from_ps(pks[:, :], rq[:, :], 128)
            yield
            k.stt(V(kvn_t[n], kvn.t[:, sl]), pk[:, :], col(l, "kv_norm", 0), rq[:, :], ALU.mult, ALU.mult)
            yield
            pr = bk.next()
            for c in range(8):
                k.mm(pr[0:96, :], winb[:, c, 832:928], z[:, c, :], start=(c == 0), stop=(c == 7))
            k.copy(V(kr_t[n], krope.t[64:96, sl]), pr[64:96, :], eng='act')

        interleave([(lambda n=n: p1_body(n)) for n in range(NT)], depth=COOP_DEPTH)
        k.pop()
        if "p1" in dbg and stop_after == "p1" and l == dbg.get("_layer", 0):
            d_ = di["dbg_p1"]
            for m in range(4):
                k.dma('sp', V(d_, d_.t[m * 128:(m + 1) * 128, :]), V(uT, uT.t[:, m, :]), allow_cast=True) if False else None
            tmpd = k.sb("tmpd", [128, T], F32)
            for m in range(4):
                k.copy(tmpd[:, :], V(uT, uT.t[:, m, :]))
                k.dma('sp', V(d_, d_.t[m * 128:(m + 1) * 128, :]), tmpd[:, :])
            for m in range(2):
                k.copy(tmpd[:, :], V(qn, qn.t[:, m, :]))
                k.dma('sp', V(d_, d_.t[512 + m * 128:512 + (m + 1) * 128, :]), tmpd[:, :])
            k.copy(tmpd[:, :], V(kvn, kvn.t[:, :]))
            k.dma('sp', V(d_, d_.t[768:896, :]), tmpd[:, :])
            k.dma('pool', V(d_, d_.t[896:928, :]), V(krope, krope.t[64:96, :]))
        if stop_after == "p1":
            k.pop()
            k.pop()
            return False
        mla_phase(l, qn, qn_t, kvn, kvn_t, krope, kr_t)
        k.pop()
        if stop_after == "mla":
            k.pop()
            return False
        mla_out(l, Hin, Hmid)
        if stop_after == "mlaout":
            k.pop()
            return True
        ssm_phase(l, uT, uT_t, Hmid, Hmid)
        k.pop()
        return True

    def ssm_phase(l, uT, uT_t, Hin, Hmid):
        k.push()
        yss = k.sb("yss", [128, 4, T], BF16)
        yss_c = [Buf(f"yss{c}", yss.t) for c in range(NCH)]
        k.push()
        Bbd = k.sb("Bbd", [128, 4, 512], BF16)
        Cbd = k.sb("Cbd", [128, 16, 2, 128], BF16)
        WA = k.sb("WA", [128, 4096], F32)
        WB = k.sb("WB", [128, 4096], F32)
        Pre = k.sb("Pre", [128, 16, 128], F32)
        Pim = k.sb("Pim", [128, 16, 128], F32)
        Preb = k.sb("Preb", [128, 16, 128], BF16)
        Pimb = k.sb("Pimb", [128, 16, 128], BF16)
        nPimb = k.sb("nPimb", [128, 16, 128], BF16)
        k.dma('pool', Cbd[:, :, :, :], V(Cbd_d, Cbd_d.t[l].rearrange("p (a b c) -> p a b c", a=16, b=2)))
        k.push()
        tA = k.sb("tA", [128, 4096], F32)
        tB = k.sb("tB", [128, 4096], F32)
        tC = k.sb("tC", [128, 4096], F32)
        tD = WA
        tE = WB
        tI = k.sb("tI", [128, 4096], mybir.dt.int32)
        j1 = cols[:, C_J1:C_J1 + 1]
        h2 = slice(0, 2048)
        k.dma('sp', tA[:, h2], V(bbLs, bbLs.t[l]))
        k.dma('sp', tB[:, h2], V(bbA_re, bbA_re.t[l]))
        k.dma('sp', tC[:, h2], V(bbA_im, bbA_im.t[l]))
        k.act(tA[:, h2], tA[:, h2], AF.Exp)
        k.tt(tD[:, h2], tB[:, h2], tA[:, h2], ALU.mult)
        k.tt(tE[:, h2], tC[:, h2], tA[:, h2], ALU.mult)
        k.act(tD[:, h2], tD[:, h2], AF.Exp)
        h3 = slice(2048, 4096)
        sincos(tE[:, h2], tA[:, h3], tD[:, h3], tA[:, h2], tI[:, h2])
        k.tt(tD[:, h3], tD[:, h3], tD[:, h2], ALU.mult)
        k.ts(tD[:, h3], tD[:, h3], -1.0, ALU.add)
        k.tt(tA[:, h3], tA[:, h3], tD[:, h2], ALU.mult)
        k.tt(tA[:, h2], tB[:, h2], tB[:, h2], ALU.mult)
        k.tt(tD[:, h2], tC[:, h2], tC[:, h2], ALU.mult)
        k.tt(tA[:, h2], tA[:, h2], tD[:, h2], ALU.add)
        k.op('dve', lambda e: e.reciprocal(out=tA.t[:, h2], in_=tA.t[:, h2]), [tA[:, h2]], [tA[:, h2]])
        k.tt(tE[:, h2], tD[:, h3], tB[:, h2], ALU.mult)
        k.tt(tE[:, h3], tA[:, h3], tC[:, h2], ALU.mult)
        k.tt(tE[:, h2], tE[:, h2], tE[:, h3], ALU.add)
        k.tt(tE[:, h2], tE[:, h2], tA[:, h2], ALU.mult)
        k.tt(tE[:, h3], tA[:, h3], tB[:, h2], ALU.mult)
        k.tt(tD[:, h2], tD[:, h3], tC[:, h2], ALU.mult)
        k.tt(tE[:, h3], tE[:, h3], tD[:, h2], ALU.subtract)
        k.tt(tE[:, h3], tE[:, h3], tA[:, h2], ALU.mult)
        def slot(buf, hs, r):
            a = buf.t[:, hs].rearrange("p (a r c) -> p a r c", r=2, c=128)
            return V(buf, a[:, :, r, :])
        k.dma('sp', tB[:, h2], V(BRd, BRd.t[l]))
        k.dma('sp', tC[:, h2], V(BId, BId.t[l]))
        k.tt(slot(tA, h3, 0), slot(tB, h2, 0), slot(tE, h2, 0), ALU.mult)
        k.tt(slot(tD, h3, 0), slot(tC, h2, 0), slot(tE, h3, 0), ALU.mult)
        k.tt(slot(tA, h3, 0), slot(tA, h3, 0), slot(tD, h3, 0), ALU.subtract)
        k.tt(slot(tA, h3, 1), slot(tB, h2, 1), slot(tE, h3, 1), ALU.mult)
        k.tt(slot(tD, h3, 1), slot(tC, h2, 1), slot(tE, h2, 1), ALU.mult)
        k.tt(slot(tA, h3, 1), slot(tA, h3, 1), slot(tD, h3, 1), ALU.add)
        k.copy(V(Bbd, Bbd.t[:, :, :].rearrange("p a b -> p (a b)")), tA[:, h3])
        fo = CP["fA_re"][0] + l * CPL
        fi = CP["fA_im"][0] + l * CPL
        fl = CP["fLs"][0] + l * CPL
        st16 = tB[:, 0:16]
        r16 = tB[:, 16:32]
        th16 = tB[:, 32:48]
        k.act(st16, cols[:, fl:fl + 16], AF.Exp)
        k.tt(r16, cols[:, fo:fo + 16], st16, ALU.mult)
        k.tt(th16, cols[:, fi:fi + 16], st16, ALU.mult)
        if "ssmtab2" in dbg:
            k.dma('sp', V(di["dbg_ssmtab2"], di["dbg_ssmtab2"].t[:, 0:48]), tB[:, 0:48])
            k.dma('sp', V(di["dbg_ssmtab2"], di["dbg_ssmtab2"].t[:, 64:64 + NCOLS]), cols[:, :])
        PR = tC.t[:, 0:2048].rearrange("p (a t) -> p a t", t=128)
        PA = tD.t[:, 0:2048].rearrange("p (a t) -> p a t", t=128)
        t1b = V(cst, t1row.ap.unsqueeze(1).to_broadcast([128, 16, 128]))
        k.tt(V(tC, PR), t1b, V(tB, r16.ap.unsqueeze(2).to_broadcast([128, 16, 128])), ALU.mult)
        k.act(tC[:, 0:2048], tC[:, 0:2048], AF.Exp)
        k.tt(V(tD, PA), t1b, V(tB, th16.ap.unsqueeze(2).to_broadcast([128, 16, 128])), ALU.mult)
        sincos(tD[:, 0:2048], tE[:, 0:2048], tE[:, 2048:4096], tA[:, 0:2048], tI[:, 0:2048])
        k.tt(V(Pre, Pre.t[:, :, :].rearrange("p a t -> p (a t)")), tC[:, 0:2048], tE[:, 2048:4096], ALU.mult)
        k.tt(V(Pim, Pim.t[:, :, :].rearrange("p a t -> p (a t)")), tC[:, 0:2048], tE[:, 0:2048], ALU.mult)
        fl2 = lambda b_: V(b_, b_.t[:, :, :].rearrange("p a t -> p (a t)"))
        k.ts(fl2(nPimb), fl2(Pim), -1.0, ALU.mult)
        k.copy(fl2(Preb), fl2(Pre))
        k.copy(fl2(Pimb), fl2(Pim))
        k.dma('sp', tA[:, :], V(rowLs, rowLs.t[l]))
        k.dma('sp', tB[:, :], V(rowA_re, rowA_re.t[l]))
        k.dma('sp', tC[:, :], V(rowA_im, rowA_im.t[l]))
        k.act(tA[:, :], tA[:, :], AF.Exp)
        k.tt(tB[:, :], tB[:, :], tA[:, :], ALU.mult)
        k.tt(tC[:, :], tC[:, :], tA[:, :], ALU.mult)
        k.ts(tB[:, :], tB[:, :], j1, ALU.mult, -1.0, ALU.mult)
        k.act(tB[:, :], tB[:, :], AF.Exp)
        k.ts(tC[:, :], tC[:, :], j1, ALU.mult)
        sincos(tC[:, :], WB[:, :], WA[:, :], tA[:, :], tI[:, :])
        k.tt(WA[:, :], tB[:, :], WA[:, :], ALU.mult)
        k.tt(WB[:, :], tB[:, :], WB[:, :], ALU.mult)
        wb4 = WB.t[:, :].rearrange("p (a r c) -> p a r c", r=2, c=128)
        k.ts(V(WB, wb4[:, :, 1, :]), V(WB, wb4[:, :, 1, :]), -1.0, ALU.mult)
        if "ssmtab" in dbg:
            d_ = di["dbg_ssmtab"]
            k.dma('sp', V(d_, d_.t[:, 0:4096]), WA[:, :])
            k.dma('sp', V(d_, d_.t[:, 4096:8192]), WB[:, :])
            k.dma('sp', V(d_, d_.t[:, 8192:10240]), V(Pre, Pre.t[:, :, :].rearrange("p a t -> p (a t)")))
            k.dma('sp', V(d_, d_.t[:, 10240:12288]), V(Pim, Pim.t[:, :, :].rearrange("p a t -> p (a t)")))
            k.copy(tA[:, 0:2048], V(Bbd, Bbd.t[:, :, :].rearrange("p a b -> p (a b)")))
            k.dma('sp', V(d_, d_.t[:, 12288:14336]), tA[:, 0:2048])
        k.pop()
        if stop_after == "ssmtab":
            k.pop()
            k.pop()
            return
        k.push()
        T1 = [k.sb("T1", [128, 4096], BF16) for _ in range(2)]
        T2 = [k.sb("T2", [128, 4096], BF16) for _ in range(2)]
        zr = k.sb("zr", [128, 1024], BF16)
        zin = k.sb("zin", [128, 1024], BF16)
        czr = k.sb("czr", [128, 8], F32)
        czi = k.sb("czi", [128, 8], F32)
        Q1 = [k.sb("Q1", [128, 8, 128], BF16) for _ in range(2)]
        Q2 = [k.sb("Q2", [128, 8, 128], BF16) for _ in range(2)]
        Q3 = [k.sb("Q3", [128, 8, 128], BF16) for _ in range(2)]
        Q4 = [k.sb("Q4", [128, 8, 128], BF16) for _ in range(2)]
        u1 = k.sb("u1", [128, 8], F32)
        u2 = k.sb("u2", [128, 8], F32)
        u3 = k.sb("u3", [128, 8], F32)
        u4 = k.sb("u4", [128, 8], F32)
        cre = k.sb("cre", [128, 16], F32)
        cim = k.sb("cim", [128, 16], F32)
        ncim = k.sb("ncim", [128, 16], F32)
        k.memset(cre[:, :], 0.0)
        k.memset(cim[:, :], 0.0)
        k.memset(ncim[:, :], 0.0)

        def genA(c):
            n = c // 4
            cs = slice(c * 128, (c + 1) * 128)
            t1, t2 = T1[c % 2], T2[c % 2]
            for beta in range(8):
                pv = psb[beta % 2]
                kap, eta = beta // 2, beta % 2
                rs = slice(64 * eta, 64 * eta + 64)
                k.mm(pv[:, :], V(uT_t[n], uT.t[rs, kap, cs]), Bbd[rs, kap, :])
                yield
                ws = slice(beta * 512, (beta + 1) * 512)
                k.tt(t1[:, ws], pv[:, :], WA[:, ws], ALU.mult)
                yield
                p4 = pv.t[:, :].rearrange("p (a r c) -> p a r c", r=2, c=128)
                w4 = WB.t[:, ws].rearrange("p (a r c) -> p a r c", r=2, c=128)
                o4 = t2.t[:, ws].rearrange("p (a r c) -> p a r c", r=2, c=128)
                k.tt(V(t2, o4[:, :, 0, :]), V(pv, p4[:, :, 1, :]), V(WB, w4[:, :, 0, :]), ALU.mult)
                yield
                k.tt(V(t2, o4[:, :, 1, :]), V(pv, p4[:, :, 0, :]), V(WB, w4[:, :, 1, :]), ALU.mult)
                yield

        def genB(c):
            n = c // 4
            cs = slice(c * 128, (c + 1) * 128)
            t1, t2 = T1[c % 2], T2[c % 2]
            for hf in range(2):
                pzr = [psb[2], psb[3]]
                pzi = [psb[4], psb[5]]
                for pp in range(8):
                    pair = hf * 8 + pp
                    base = pair * 256
                    orr = pzr[pp // 4][:, (pp % 4) * 128:(pp % 4 + 1) * 128]
                    oii = pzi[pp // 4][:, (pp % 4) * 128:(pp % 4 + 1) * 128]
                    k.mm(orr, t1[:, base:base + 128], tri_b, start=True, stop=False, inc=False)
                    k.mm(orr, t2[:, base:base + 128], tri_b, start=False, stop=True, inc=(pp % 4 == 3))
                    k.mm(oii, t1[:, base + 128:base + 256], tri_b, start=True, stop=False, inc=False)
                    k.mm(oii, t2[:, base + 128:base + 256], tri_b, start=False, stop=True, inc=(pp % 4 == 3))
                    yield
                ps8 = slice(hf * 8, hf * 8 + 8)
                for i in range(2):
                    fs = slice(i * 512, (i + 1) * 512)
                    cb_r = V(cre, cre.t[:, hf * 8 + i * 4:hf * 8 + i * 4 + 4].unsqueeze(2).to_broadcast([128, 4, 128]))
                    cb_i = V(cim, cim.t[:, hf * 8 + i * 4:hf * 8 + i * 4 + 4].unsqueeze(2).to_broadcast([128, 4, 128]))
                    zr3 = V(zr, zr.t[:, fs].rearrange("p (a t) -> p a t", t=128))
                    zi3 = V(zin, zin.t[:, fs].rearrange("p (a t) -> p a t", t=128))
                    pr3 = V(pzr[i], pzr[i].t[:, :].rearrange("p (a t) -> p a t", t=128))
                    pi3 = V(pzi[i], pzi[i].t[:, :].rearrange("p (a t) -> p a t", t=128))
                    k.tt(zr3, pr3, cb_r, ALU.add)
                    yield
                    k.stt(zi3, pi3, -1.0, cb_i, ALU.mult, ALU.subtract)
                    yield
                for i in range(2):
                    lr = V(pzr[i], pzr[i].t[:, :].rearrange("p (a t) -> p a t", t=128)[:, :, 127])
                    li = V(pzi[i], pzi[i].t[:, :].rearrange("p (a t) -> p a t", t=128)[:, :, 127])
                    o_r, c_r = czr[:, i * 4:(i + 1) * 4], cre[:, hf * 8 + i * 4:hf * 8 + i * 4 + 4]
                    k.op('dve', lambda e, o_r=o_r, lr=lr, c_r=c_r: e.tensor_tensor(out=o_r.ap, in0=lr.ap, in1=c_r.ap, op=ALU.add),
                         [lr, c_r, zr[:, :], zin[:, :]], [o_r])
                    yield
                    o_i, c_i = czi[:, i * 4:(i + 1) * 4], cim[:, hf * 8 + i * 4:hf * 8 + i * 4 + 4]
                    k.op('dve', lambda e, o_i=o_i, li=li, c_i=c_i: e.tensor_tensor(out=o_i.ap, in0=li.ap, in1=c_i.ap, op=ALU.add),
                         [li, c_i, zr[:, :], zin[:, :]], [o_i])
                    yield
                PreH = V(Preb, Preb.t[:, ps8, :].rearrange("p a t -> p (a t)"))
                PimH = V(Pimb, Pimb.t[:, ps8, :].rearrange("p a t -> p (a t)"))
                nPimH = V(nPimb, nPimb.t[:, ps8, :].rearrange("p a t -> p (a t)"))
                fl_ = lambda b_: V(b_, b_.t[:, :, :].rearrange("p a t -> p (a t)"))
                k.tt(fl_(Q1[hf]), zr[:, :], PreH, ALU.mult)
                yield
                k.tt(fl_(Q3[hf]), zr[:, :], nPimH, ALU.mult)
                yield
                k.tt(fl_(Q2[hf]), zin[:, :], PimH, ALU.mult)
                yield
                k.tt(fl_(Q4[hf]), zin[:, :], PreH, ALU.mult)
                yield
                lPre = V(Pre, Pre.t[:, ps8, 127])
                lPim = V(Pim, Pim.t[:, ps8, 127])
                k.tt(u1[:, :], czr[:, :], lPre, ALU.mult)
                yield
                k.tt(u2[:, :], czi[:, :], lPim, ALU.mult)
                yield
                k.tt(u3[:, :], czr[:, :], lPim, ALU.mult)
                yield
                k.tt(u4[:, :], czi[:, :], lPre, ALU.mult)
                yield
                k.tt(cre[:, ps8], u1[:, :], u2[:, :], ALU.subtract)
                yield
                k.tt(cim[:, ps8], u3[:, :], u4[:, :], ALU.add)
                yield
                k.ts(ncim[:, ps8], cim[:, ps8], -1.0, ALU.mult)
                yield
            py = psb[6 + c % 2]
            for kap in range(4):
                for pp in range(4):
                    pair = kap * 4 + pp
                    hf, p8 = pair // 8, pair % 8
                    o_ = py[:, kap * 128:(kap + 1) * 128]
                    k.mm(o_, Cbd[:, pair, 0, :], Q1[hf][:, p8, :], start=(pp == 0), stop=False, inc=False)
                    k.mm(o_, Cbd[:, pair, 0, :], Q2[hf][:, p8, :], start=False, stop=False, inc=False)
                    k.mm(o_, Cbd[:, pair, 1, :], Q3[hf][:, p8, :], start=False, stop=False, inc=False)
                    k.mm(o_, Cbd[:, pair, 1, :], Q4[hf][:, p8, :], start=False, stop=(pp == 3), inc=(pp == 3))
                yield
            for kap in range(4):
                k.stt(V(yss_c[c], yss.t[:, kap, cs]), V(uT_t[n], uT.t[:, kap, cs]), col(l, "ssm_D", kap),
                      py[:, kap * 128:(kap + 1) * 128], ALU.mult, ALU.add)
                yield

        for _ in genA(0):
            pass
        for c in range(NCH):
            gb = genB(c)
            ga = genA(c + 1) if c + 1 < NCH else iter(())
            alive = True
            while alive:
                alive = False
                for g_ in (gb, ga):
                    try:
                        next(g_)
                        alive = True
                    except StopIteration:
                        pass
        k.pop()
        k.pop()
        if stop_after == "ssmloop":
            k.push()
            tmpd = k.sb("tmpd", [128, T], F32)
            d_ = di["dbg_yssm"]
            for m in range(4):
                k.op('dve', lambda e, m=m: e.tensor_copy(out=tmpd.t[:, :], in_=yss.t[:, m, :]), [V(b_, yss.t) for b_ in yss_c], [tmpd[:, :]])
                k.dma('sp', V(d_, d_.t[m * 128:(m + 1) * 128, :]), tmpd[:, :])
            k.pop()
            k.pop()
            return
        k.push()
        wglu = k.sb("wglu", [128, 4, 512], BF16)
        k.dma('pool', wglu[:, :, :], V(w_glu, w_glu.t[l].rearrange("(c p) n -> p c n", p=128)))
        wo = k.sb("wo", [128, 4, D], BF16)
        k.dma('pool', wo[:, :, :], V(w_out, w_out.t[l, 0:512, :].rearrange("(c p) n -> p c n", p=128)))
        yg = [k.sb("yg", [128, 4, TS], BF16) for _ in range(2)]
        yf = [k.sb("yf", [128, 4, TS], F32) for _ in range(2)]
        e1_2 = [k.sb("e1", [128, TS], F32) for _ in range(2)]
        e2_2 = [k.sb("e2", [128, TS], F32) for _ in range(2)]
        sg_2 = [k.sb("sg", [128, TS], F32) for _ in range(2)]
        sq = [[k.sb("sq2", [128, TS], BF16) for _ in range(2)] for _ in range(2)]
        rs2 = [k.sb("rs_", [128, TS], F32) for _ in range(2)]
        mx = [k.sb("mx", [128, 4, TS], BF16) for _ in range(2)]
        hb = [k.sb("hb2", [128, 8, TS], F32) for _ in range(2)]

        def glu_body(n):
            par = n % 2
            bset = psb[3 * par:3 * par + 3]
            alt = Banks(bset[1:3])
            e1, e2, sg, rs_ = e1_2[par], e2_2[par], sg_2[par], rs2[par]
            sl = slice(n * TS, (n + 1) * TS)
            ygn, yfn, mxn, hbn = yg[par], yf[par], mx[par], hb[par]
            k.dma('sp', hbn[:, :, :], hview(Hin, n))
            yield
            for c4 in range(4):
                yv = V(yss_c[n * 4], yss.t[:, c4, sl])
                ins_extra = [V(yss_c[n * 4 + i], yss.t[:, c4, sl]) for i in range(1, 4)]
                k.op('dve', lambda e, yv=yv: e.tensor_tensor(out=e1.t[:, :], in0=yv.ap, in1=yv.ap, op=ALU.mult), [yv] + ins_extra, [e1[:, :]])
                k.ts(e1[:, :], e1[:, :], 0.044715, ALU.mult, 1.0, ALU.add)
                yield
                k.tt(e1[:, :], e1[:, :], yv, ALU.mult)
                k.act(e2[:, :], e1[:, :], AF.Sigmoid, scale=1.5957691216057308)
                yield
                k.tt(yfn[:, c4, :], e2[:, :], yv, ALU.mult)
                k.copy(ygn[:, c4, :], yfn[:, c4, :], eng='act')
                yield
            pss = bset[0]
            for m in range(4):
                pg = alt.next()
                for c4 in range(4):
                    k.mm(pg[:, :], wglu[:, c4, m * 128:(m + 1) * 128], ygn[:, c4, :], start=(c4 == 0), stop=(c4 == 3))
                k.act(sg[:, :], pg[:, :], AF.Sigmoid, bias=col(l, "b_glu", m))
                yield
                k.tt(yfn[:, m, :], yfn[:, m, :], sg[:, :], ALU.mult)
                k.act(sq[par][m % 2][:, :], yfn[:, m, :], AF.Square)
                k.mm(pss[:, :], ones_b, sq[par][m % 2][:, :], start=(m == 0), stop=(m == 3), inc=True)
                yield
            rstd_from_ps(pss[:, :], rs_[:, :], 512)
            yield
            for m in range(4):
                k.stt(mxn[:, m, :], yfn[:, m, :], col(l, "ssm_out_norm", m), rs_[:, :], ALU.mult, ALU.mult)
            yield
            if "yssm" in dbg and l == dbg.get("_layer", 0):
                for m in range(4):
                    k.dma('sp', V(di["dbg_yssm"], di["dbg_yssm"].t[m * 128:(m + 1) * 128, sl]), yfn[:, m, :])
            for m in range(8):
                po = alt.next()
                for c4 in range(4):
                    k.mm(po[:, :], wo[:, c4, m * 128:(m + 1) * 128], mxn[:, c4, :], start=(c4 == 0), stop=(c4 == 3))
                k.tt(hbn[:, m, :], hbn[:, m, :], po[:, :], ALU.add)
                yield
            k.dma('sp', hview(Hmid, n), hbn[:, :, :])
            yield

        interleave([(lambda n=n: glu_body(n)) for n in range(NT)], depth=COOP_DEPTH)
        k.pop()
        k.pop()

    def mla_phase(l, qn, qn_t, kvn, kvn_t, krope, kr_t):
        k.push()
        rC = k.sb("rC", [96, T], F32)
        rS = k.sb("rS", [96, T], F32)
        k.dma('sp', rC[:, :], di["ropeC"][:, :])
        k.dma('sp', rS[:, :], di["ropeS"][:, :])
        wq = k.sb("wq", [128, 2, 768], BF16)
        k.dma('pool', wq[:, :, :], V(w_q_up, w_q_up.t[l].rearrange("(c p) n -> p c n", p=128)))
        wkv = k.sb("wkv", [128, 1024], BF16)
        k.dma('pool', wkv[:, :], V(w_kv_up, w_kv_up.t[l]))
        Vt = k.sb("Vt", [128, NCH, 8, 65], BF16)
        k.memset(V(Vt, Vt.t[:, :, :, 64:65]), 1.0)
        wkv_v = V(wkv, wkv.t[:, :].rearrange("p (h x) -> p h x", x=128)[:, :, 64:128])
        for c in range(NCH):
            n = c // 4
            pv = nps()
            k.mm(V(pv, pv.t[:, :].rearrange("p (h x) -> p h x", x=64)), V(kvn_t[n], kvn.t[:, c * 128:(c + 1) * 128]), wkv_v)
            k.copy(V(Vt, Vt.t[:, c, :, 0:64]), V(pv, pv.t[:, :].rearrange("p (h x) -> p h x", x=64)), eng=('act' if c % 2 else 'dve'))
        QT = [k.sb("QT", [96, T], BF16) for _ in range(2)]
        KT = [k.sb("KT", [96, T], BF16) for _ in range(2)]
        kpre = k.sb("kpre", [96, TS], F32)
        sqh = k.sb("sqh", [96, TS], BF16)
        rh = k.sb("rh", [96, TS], F32)
        xg = k.sb("xg", [96, TS], F32)
        xgb = k.sb("xgb", [96, TS], BF16)
        xr1 = k.sb("xr1", [96, TS], F32)
        xr2 = k.sb("xr2", [96, TS], F32)
        PT = [k.sb("PT", [128, TS], BF16) for _ in range(4)]
        rsum = k.sb("rsum", [128, TS], F32)
        rbc = k.sb("rbc", [64, TS], F32)
        osb = [k.sb("osb", [64, TS], F32) for _ in range(2)]
        ones96 = cstb[0:96, 512:608]
        ones_r64 = cstb[64:65, 512:576]
        O_h = O_state["O_h"]
        O_t = [[O_h[h]] * NT for h in range(8)]
        rot2 = [0]

        def nps2():
            rot2[0] += 1
            return psb[3 + rot2[0] % 3]

        BQ = dict(sqh=k.sb("sqhq", [96, TS], BF16), rh=k.sb("rhq", [96, TS], F32), xg=k.sb("xgq", [96, TS], F32),
                  xgb=k.sb("xgbq", [96, TS], BF16), xr1=k.sb("xr1q", [96, TS], F32), xr2=k.sb("xr2q", [96, TS], F32))
        BK = dict(sqh=sqh, rh=rh, xg=xg, xgb=xgb, xr1=xr1, xr2=xr2, pre=kpre, bank=psb[4])
        BQ["pre"] = k.sb("qpre", [96, TS], F32)
        BQ["bank"] = psb[3]

        def chain(h, n, which, B):
            Q, Kh = QT[h % 2], KT[h % 2]
            sqh_, rh_, xg_, xgb_, xr1_, xr2_ = B["sqh"], B["rh"], B["xg"], B["xgb"], B["xr1"], B["xr2"]
            sl = slice(n * TS, (n + 1) * TS)
            pre = B["pre"]
            pp = B["bank"]
            if which == 0:
                for c2 in range(2):
                    k.mm(pp[0:96, :], wq[:, c2, h * 96:(h + 1) * 96], V(qn_t[n], qn.t[:, c2, sl]), start=(c2 == 0), stop=(c2 == 1))
                yield
                k.copy(pre[:, :], pp[0:96, :], eng='dve')
                yield
                gain = col(l, "q_gain", 0, 96)
                dst = Q
            else:
                k.mm(pp[0:64, :], wkv[:, h * 128:h * 128 + 64], V(kvn_t[n], kvn.t[:, sl]))
                yield
                k.copy(pre[0:64, :], pp[0:64, :], eng='dve')
                yield
                k.copy(pre[64:96, :], V(kr_t[n], krope.t[64:96, sl]), eng='dve')
                yield
                gain = col(l, "k_gain", 0, 96)
                dst = Kh
            src = pre[:, :]
            k.tt(sqh_[:, :], src, src, ALU.mult)
            yield
            pss = pp
            k.mm(pss[0:96, :], ones96, sqh_[:, :])
            yield
            k.ts(rh_[:, :], pss[0:96, :], 1.0 / 96, ALU.mult, EPS, ALU.add)
            yield
            k.act(rh_[:, :], rh_[:, :], AF.Ln)
            yield
            k.act(rh_[:, :], rh_[:, :], AF.Exp, scale=-0.5)
            yield
            k.stt(xg_[:, :], src, gain, rh_[:, :], ALU.mult, ALU.mult)
            yield
            k.copy(xgb_[:, :], xg_[:, :], eng='dve')
            yield
            psw = pp
            k.mm(psw[0:96, :], swapT_b, xgb_[:, :])
            yield
            k.tt(xr1_[:, :], xg_[:, :], rC[:, sl], ALU.mult)
            yield
            k.tt(xr2_[:, :], psw[0:96, :], rS[:, sl], ALU.mult)
            yield
            k.tt(dst[:, sl], xr1_[:, :], xr2_[:, :], ALU.add)
            yield

        def prep_gen(h):
            for n in range(NT):
                gens = [chain(h, n, 0, BQ), chain(h, n, 1, BK)]
                while gens:
                    for g_ in list(gens):
                        try:
                            next(g_)
                            yield
                        except StopIteration:
                            gens.remove(g_)

        for _ in prep_gen(0):
            pass
        LOOK = 2
        SC = float(96 ** -0.5)
        for h in range(8):
            Q, Kh = QT[h % 2], KT[h % 2]
            gen = prep_gen(h + 1) if h < 7 else iter(())
            iters = [(sbq, kb) for sbq in range(NT) for kb in range(4 * sbq + 4)]
            Sinfo = {}

            def emit_S(i):
                sbq, kb = iters[i]
                r = kb - 4 * sbq
                q0 = sbq * TS + (max(r, 0)) * 128
                q1_ = (sbq + 1) * TS
                w = q1_ - q0
                pS = psb[i % 3]
                k.mm(pS[:, 0:w], Kh[:, kb * 128:(kb + 1) * 128], Q[:, q0:q1_])
                Sinfo[i] = (pS, w, r, q0 - sbq * TS)

            for i in range(min(LOOK, len(iters))):
                emit_S(i)
            for i, (sbq, kb) in enumerate(iters):
                if i + LOOK < len(iters):
                    emit_S(i + LOOK)
                pS, w, r, off = Sinfo.pop(i)
                nkb = 4 * sbq + 4
                po = psb[6 + sbq % 2]
                pt = PT[i % 4]
                if r >= 0:
                    k.act(pt[:, 0:64], pS[:, 0:64], AF.Exp, scale=SC, bias=cst[:, 480:481])
                    k.act(pt[:, 64:w], pS[:, 64:w], AF.Exp, scale=SC)
                else:
                    k.act(pt[:, 0:w], pS[:, 0:w], AF.Exp, scale=SC)
                k.mm(po[0:65, off:TS], Vt[:, kb, h, :], pt[:, 0:w], start=(kb == 0), stop=(kb == nkb - 1), inc=True)
                if kb == nkb - 1:
                    k.act(rsum[64:65, :], po[64:65, :], AF.Ln)
                    k.act(rsum[64:65, :], rsum[64:65, :], AF.Exp, scale=-1.0)
                    pb = psb[5]
                    k.mm(pb[0:64, :], cst[64:65, 512:576], rsum[64:65, :])
                    k.copy(rbc[:, :], pb[0:64, :], eng='dve')
                    ob = osb[sbq % 2]
                    k.tt(ob[:, :], po[0:64, :], rbc[:, :], ALU.mult)
                    k.dma('sp', V(O_t[h][sbq], oscr.t[h, :, sbq * TS:(sbq + 1) * TS]), ob[:, :])
                next(gen, None)
                next(gen, None)
            for _ in gen:
                pass
        k.pop()

    def mla_out(l, Hin, Hmid):
        O_h = O_state["O_h"]
        O_t = [[O_h[h]] * NT for h in range(8)]
        k.push()
        wo = k.sb("wo2", [64, 8, D], BF16)
        k.dma('pool', wo[:, :, :], V(w_out, w_out.t[l, 512:1024, :].rearrange("(h p) n -> p h n", p=64)))
        ot = [k.sb("ot", [64, 8, TS], F32) for _ in range(2)]
        otl = [[Buf(f"otl{i}_{h}", ot[i].t) for h in range(8)] for i in range(2)]
        sq = [[k.sb("sq3", [64, TS], BF16) for _ in range(2)] for _ in range(2)]
        rs2 = [k.sb("rs3", [64, TS], F32) for _ in range(2)]
        mx = [k.sb("mx3", [64, 8, TS], BF16) for _ in range(2)]
        hb = [k.sb("hb3", [128, 8, TS], F32) for _ in range(2)]
        ones64 = cstb[0:64, 512:576]

        def mo_body(n):
            par = n % 2
            bk = Banks(psb[3 * par:3 * par + 3])
            rs_ = rs2[par]
            sl = slice(n * TS, (n + 1) * TS)
            otn, mxn, hbn = ot[par], mx[par], hb[par]
            k.dma('sp', hbn[:, :, :], hview(Hin, n))
            yield
            pss = bk.next()
            for h in range(8):
                ov = V(otl[par][h], otn.t[:, h, :])
                k.dma('sp', ov, V(O_t[h][n], oscr.t[h, :, sl]))
                k.act(sq[par][h % 2][:, :], ov, AF.Square)
                k.mm(pss[0:64, :], ones64, sq[par][h % 2][:, :], start=(h == 0), stop=(h == 7), inc=True)
                if h % 2 == 1:
                    yield
            rstd_from_ps(pss[0:64, :], rs_[:, :], 512)
            yield
            for h in range(8):
                ov = V(otl[par][h], otn.t[:, h, :])
                k.stt(mxn[:, h, :], ov, col(l, "mla_out_norm", h, 64), rs_[:, :], ALU.mult, ALU.mult)
                if h % 4 == 3:
                    yield
            if "ymla" in dbg and l == dbg.get("_layer", 0):
                for h in range(8):
                    k.dma('sp', V(di["dbg_ymla"], di["dbg_ymla"].t[h * 64:(h + 1) * 64, sl]), V(otl[par][h], otn.t[:, h, :]))
            for m in range(8):
                po = bk.next()
                for h in range(8):
                    k.mm(po[:, :], wo[:, h, m * 128:(m + 1) * 128], mxn[:, h, :], start=(h == 0), stop=(h == 7))
                k.tt(hbn[:, m, :], hbn[:, m, :], po[:, :], ALU.add)
                yield
            k.dma('sp', hview(Hmid, n), hbn[:, :, :])
            yield

        interleave([(lambda n=n: mo_body(n)) for n in range(NT)], depth=COOP_DEPTH)
        k.pop()

    def ffn_layer(l, Hmid, Hout, moe):
        k.push()
        F = 3584 if moe else 2816
        NE = 8 if moe else 1
        GS = 4
        nfc = F // 128
        groups = [(g0, min(GS, nfc - g0)) for g0 in range(0, nfc, GS)]
        HT = 2048
        gates_tm = k.sb("gates", [128, NCH, 8], F32) if moe else None
        if moe:
            wr = k.sb("wr", [128, 8, 8], F32)
            k.dma('sp', wr[:, :, :], V(mwr, mwr.t[0].rearrange("(c p) e -> p c e", p=128)))
        for half in range(2):
            k.push()
            zT = k.sb("zT", [128, 8, HT], BF16)
            acc = k.sb("acc", [128, 8, HT], F32)
            zT_t = [Buf(f"zT{i}", zT.t) for i in range(4)]
            acc_t = [[Buf(f"acc{i}_{m}", acc.t) for m in range(8)] for i in range(4)]
            k.push()
            hbuf = [k.sb("hbuf", [128, 8, TS], F32) for _ in range(2)]
            sq = [[k.sb("sq", [128, TS], BF16) for _ in range(2)] for _ in range(2)]
            rstd2 = [k.sb("rstd", [128, TS], F32) for _ in range(2)]
            zf2 = [k.sb("zf", [128, 8, TS], F32) for _ in range(2)] if moe else [None, None]
            rt = [dict(lg=k.sb("lg", [128, 8], F32), mx8=k.sb("mx8", [128, 8], F32), msk=k.sb("msk", [128, 8], F32),
                       ee=k.sb("ee", [128, 8], F32), nm1=k.sb("nm1", [128, 1], F32), ssum=k.sb("ssum", [128, 1], F32))
                  for _ in range(2)]

            def fn_body(i):
                par = i % 2
                bk = Banks(psb[3 * par:3 * par + 3])
                rstd, zf = rstd2[par], zf2[par]
                lg, mx8, msk, ee, nm1, ssum = (rt[par][x] for x in ("lg", "mx8", "msk", "ee", "nm1", "ssum"))
                n = half * 4 + i
                hb = hbuf[par]
                k.dma('sp', hb[:, :, :], hview(Hmid, n))
                yield
                pss = bk.next()
                for c in range(8):
                    k.act(sq[par][c % 2][:, :], hb[:, c, :], AF.Square)
                    k.mm(pss[:, :], ones_b, sq[par][c % 2][:, :], start=(c == 0), stop=(c == 7), inc=True)
                    if c % 2 == 1:
                        yield
                rstd_from_ps(pss[:, :], rstd[:, :], D)
                yield
                for c in range(8):
                    dstv = V(zT_t[i], zT.t[:, c, i * TS:(i + 1) * TS])
                    if moe:
                        k.stt(zf[:, c, :], hb[:, c, :], col(l, "ffn_norm", c), rstd[:, :], ALU.mult, ALU.mult)
                        k.copy(dstv, zf[:, c, :], eng='act')
                    else:
                        k.stt(dstv, hb[:, c, :], col(l, "ffn_norm", c), rstd[:, :], ALU.mult, ALU.mult)
                    k.copy(V(acc_t[i][c], acc.t[:, c, i * TS:(i + 1) * TS]), hb[:, c, :], eng='act')
                    if c % 2 == 1:
                        yield
                if moe:
                    for b4 in range(4):
                        blk = n * 4 + b4
                        pl = bk.next()
                        for c in range(8):
                            k.mm(pl[:, 0:8], zf[:, c, b4 * 128:(b4 + 1) * 128], wr[:, c, :], start=(c == 0), stop=(c == 7))
                        k.copy(lg[:, :], pl[:, 0:8])
                        yield
                        k.op('dve', lambda e: e.max(out=mx8.t[:, :], in_=lg.t[:, :]), [lg[:, :]], [mx8[:, :]])
                        k.ts(msk[:, :], lg[:, :], mx8[:, 1:2], ALU.is_ge)
                        k.ts(nm1[:, :], mx8[:, 0:1], -1.0, ALU.mult)
                        yield
                        k.act(ee[:, :], lg[:, :], AF.Exp, bias=nm1[:, 0:1])
                        k.tt(ee[:, :], ee[:, :], msk[:, :], ALU.mult)
                        yield
                        k.op('dve', lambda e: e.reduce_sum(out=ssum.t[:, :], in_=ee.t[:, :], axis=AX.X), [ee[:, :]], [ssum[:, :]])
                        k.op('dve', lambda e: e.reciprocal(out=ssum.t[:, :], in_=ssum.t[:, :]), [ssum[:, :]], [ssum[:, :]])
                        k.ts(V(gates_tm, gates_tm.t[:, blk, :]), ee[:, :], ssum[:, 0:1], ALU.mult)
                        yield

            interleave([(lambda i=i: fn_body(i)) for i in range(4)], depth=COOP_DEPTH)
            k.pop()
            k.push()
            wg = [k.sb("wg", [128, 8, GS * 128], BF16) for _ in range(2)]
            wu = [k.sb("wu", [128, 8, GS * 128], BF16) for _ in range(2)]
            wd = [k.sb("wd", [128, GS, D], BF16) for _ in range(2)]
            aT = [k.sb("aT", [128, GS, HT], BF16) for _ in range(2)]
            sg = [k.sb("sg", [128, TS], F32) for _ in range(2)]
            tg = [k.sb("tg", [128, TS], F32) for _ in range(2)]
            G2 = [k.sb("G", [128, HT], F32) for _ in range(2)] if moe else None
            items = [(e, g0, gn) for e in range(NE) for (g0, gn) in groups]

            def wsrc(e):
                if moe:
                    return (mwg.t[0, e], mwu.t[0, e], mwd.t[0, e]), (mwg, mwu, mwd)
                return (dwg.t[0], dwu.t[0], dwd.t[0]), (dwg, dwu, dwd)

            def issue_weights(j):
                e, g0, gn = items[j]
                b = j % 2
                (Wg, Wu, Wd), (WgB, WuB, WdB) = wsrc(e)
                fs = slice(g0 * 128, (g0 + gn) * 128)
                k.dma('pool', V(wg[b], wg[b].t[:, :, 0:gn * 128]), V(WgB, Wg[:, fs].rearrange("(c p) f -> p c f", p=128)))
                k.dma('pool', V(wu[b], wu[b].t[:, :, 0:gn * 128]), V(WuB, Wu[:, fs].rearrange("(c p) f -> p c f", p=128)))
                k.dma('pool', V(wd[b], wd[b].t[:, 0:gn, :]), V(WdB, Wd[fs, :].rearrange("(c p) n -> p c n", p=128)))

            def make_G(e):
                G = G2[e % 2]
                for b16 in range(16):
                    blk = half * 16 + b16
                    pgx = nps()
                    k.mm(pgx[:, 0:128], V(gates_tm, gates_tm.t[:, blk, e:e + 1].to_broadcast([128, 128])), ident_f)
                    k.copy(G[:, b16 * 128:(b16 + 1) * 128], pgx[:, 0:128], eng='act')

            issue_weights(0)
            if moe:
                make_G(0)
            for j, (e, g0, gn) in enumerate(items):
                b = j % 2
                if j + 1 < len(items):
                    issue_weights(j + 1)
                if moe and g0 == groups[-1][0] and e + 1 < NE:
                    make_G(e + 1)
                G = G2[e % 2] if moe else None
                a = aT[b]
                for fc in range(gn):
                    for i in range(4):
                        tsl = slice(i * TS, (i + 1) * TS)
                        pg_, pu_ = nps(), nps()
                        for c in range(8):
                            k.mm(pg_[:, :], wg[b][:, c, fc * 128:(fc + 1) * 128], V(zT_t[i], zT.t[:, c, tsl]), start=(c == 0), stop=(c == 7))
                        for c in range(8):
                            k.mm(pu_[:, :], wu[b][:, c, fc * 128:(fc + 1) * 128], V(zT_t[i], zT.t[:, c, tsl]), start=(c == 0), stop=(c == 7))
                        s_ = sg[(fc * 4 + i) % 2]
                        t_ = tg[(fc * 4 + i) % 2]
                        k.act(s_[:, :], pg_[:, :], AF.Silu)
                        if moe:
                            k.tt(t_[:, :], pu_[:, :], s_[:, :], ALU.mult)
                            k.tt(a[:, fc, tsl], t_[:, :], G[:, tsl], ALU.mult)
                        else:
                            k.tt(a[:, fc, tsl], pu_[:, :], s_[:, :], ALU.mult)
                for i in range(4):
                    tsl = slice(i * TS, (i + 1) * TS)
                    for m in range(8):
                        pd = nps()
                        for fc in range(gn):
                            k.mm(pd[:, :], wd[b][:, fc, m * 128:(m + 1) * 128], a[:, fc, tsl], start=(fc == 0), stop=(fc == gn - 1))
                        av = V(acc_t[i][m], acc.t[:, m, tsl])
                        k.tt(av, av, pd[:, :], ALU.add)
            for i in range(4):
                n = half * 4 + i
                ins_all = [V(acc_t[i][m], acc.t[:, m, i * TS:(i + 1) * TS]) for m in range(8)]
                src = V(acc_t[i][0], acc.t[:, :, i * TS:(i + 1) * TS])
                k._waits('sp', [x.b for x in ins_all], [])
                k.dma('sp', hview(Hout, n), src)
                ev = Hout[n].w
                for x in ins_all:
                    x.b.r[ev[0]] = ev[1]
            k.pop()
            k.pop()
        k.pop()

    ok0 = mixer_layer(0, H_x, H_A)
    if stop_after in ("p1", "mla", "mlaout", "ssmtab", "ssmloop"):
        final = H_A if ok0 else H_x
    elif stop_after == "mix0":
        final = H_A
    else:
        ffn_layer(0, H_A, H_B, moe=False)
        if stop_after == "ffn0":
            final = H_B
        else:
            mixer_layer(1, H_B, H_A)
            if stop_after == "mix1":
                final = H_A
            else:
                ffn_layer(1, H_A, H_O, moe=True)
                final = H_O
    if final is not H_O:
        k.push()
        tb = [k.sb("tb", [128, 8, TS], F32) for _ in range(2)]
        for n in range(NT):
            k.dma('sp', tb[n % 2][:, :, :], hview(final, n))
            k.dma('sp', hview(H_O, n), tb[n % 2][:, :, :])
        k.pop()
    k.barrier()
    k.pop()
    k.es.close()
    return nc


def host_prep(inputs):
    f = np.float32
    shared = {}
    for nm in ["w_in", "ssm_w_glu", "mla_w_q_up", "mla_w_kv_up", "w_out", "dense_w_gate", "dense_w_up",
               "dense_w_down", "moe_w_router", "moe_w_gate", "moe_w_up", "moe_w_down"]:
        shared[nm] = np.ascontiguousarray(inputs[nm], dtype=f)
    cols = np.zeros((128, NCOLS), f)

    def put(l, name, arr2d):
        o, w = CP[name]
        cols[:arr2d.shape[0], l * CPL + o:l * CPL + o + w] = arr2d
    for l in range(2):
        put(l, "attn_norm", inputs["attn_norm"][l].reshape(8, 128).T)
        put(l, "ffn_norm", inputs["ffn_norm"][l].reshape(8, 128).T)
        put(l, "q_norm", inputs["mla_q_norm"][l].reshape(2, 128).T)
        put(l, "kv_norm", inputs["mla_kv_norm"][l].reshape(1, 128).T)
        put(l, "q_gain", inputs["mla_q_gain"][l].reshape(1, 96).T)
        put(l, "k_gain", inputs["mla_k_gain"][l].reshape(1, 96).T)
        put(l, "ssm_out_norm", inputs["ssm_out_norm"][l].reshape(4, 128).T)
        put(l, "mla_out_norm", inputs["mla_out_norm"][l].reshape(8, 64).T)
        put(l, "b_glu", inputs["ssm_b_glu"][l].reshape(4, 128).T)
        put(l, "ssm_D", inputs["ssm_D"][l].reshape(4, 128).T)
        put(l, "fA_re", inputs["ssm_A_re"][l].reshape(16, 2, 64).transpose(1, 2, 0).reshape(128, 16))
        put(l, "fA_im", inputs["ssm_A_im"][l].reshape(16, 2, 64).transpose(1, 2, 0).reshape(128, 16))
        put(l, "fLs", np.broadcast_to(inputs["ssm_log_step"][l].reshape(16, 2, 1), (16, 2, 64)).transpose(1, 2, 0).reshape(128, 16))
    cols[:, C_J1] = np.arange(1, 129, dtype=f)
    shared["cols"] = cols
    def rowtab(a_gp):
        t = a_gp.reshape(16, 1, 2, 64)
        t = np.broadcast_to(t, (16, 2, 2, 64)).reshape(1, 4096)
        return np.ascontiguousarray(np.broadcast_to(t, (128, 4096)), dtype=f)
    shared["rowA_re"] = np.stack([rowtab(inputs["ssm_A_re"][l]) for l in range(2)])
    shared["rowA_im"] = np.stack([rowtab(inputs["ssm_A_im"][l]) for l in range(2)])
    shared["rowLs"] = np.stack([rowtab(np.broadcast_to(inputs["ssm_log_step"][l][:, None], (32, 64))) for l in range(2)])
    def bbtab(a_gp, l):
        t = a_gp.reshape(4, 8, 1, 1, 64)
        t = np.broadcast_to(t, (4, 8, 16, 8, 64))
        t = t.transpose(1, 2, 0, 3, 4).reshape(128, 2048)
        return np.ascontiguousarray(t, dtype=f)
    shared["bbA_re"] = np.stack([bbtab(inputs["ssm_A_re"][l], l) for l in range(2)])
    shared["bbA_im"] = np.stack([bbtab(inputs["ssm_A_im"][l], l) for l in range(2)])
    shared["bbLs"] = np.stack([bbtab(np.broadcast_to(inputs["ssm_log_step"][l][:, None], (32, 64)), l) for l in range(2)])

    def bplace(Bm):
        o = np.zeros((8, 16, 4, 2, 2, 2, 64), f)
        for g in range(32):
            kap, g8 = g // 8, g % 8
            pair2, g2 = (g8 % 4) // 2, g8 % 2
            for r in range(2):
                o[g8, :, kap, pair2, r, g2, :] = Bm[g].T
        return o.reshape(128, 2048)
    shared["BR"] = np.stack([bplace(inputs["ssm_B_re"][l]) for l in range(2)])
    shared["BI"] = np.stack([bplace(inputs["ssm_B_im"][l]) for l in range(2)])

    def cplace(Cr, Ci):
        o = np.zeros((2, 64, 16, 2, 8, 16), f)
        for g in range(32):
            pair, g2 = g // 2, g % 2
            g8 = g % 8
            o[g2, :, pair, 0, g8, :] = Cr[g].T
            o[g2, :, pair, 1, g8, :] = Ci[g].T
        return o.reshape(128, 4096)
    shared["Cbd"] = np.stack([cplace(inputs["ssm_C_re"][l], inputs["ssm_C_im"][l]) for l in range(2)])
    cst = np.zeros((128, 640), f)
    cst[:, 0:128] = np.eye(128, dtype=f)
    cst[:, 128:256] = np.triu(np.ones((128, 128), f))
    cst[:, 256:384] = np.arange(1, 129, dtype=f)[None, :]
    sw = np.zeros((96, 96), f)
    for i in range(16):
        sw[80 + i, 64 + i] = -1.0
        sw[64 + i, 80 + i] = 1.0
    cst[0:96, 384:480] = sw
    cst[:, 512:640] = 1.0
    cst[64:128, 480] = -30000.0
    shared["consts"] = cst
    inv = 1.0 / (10000.0 ** (np.arange(0, 32, 2, dtype=np.float32) / 32))
    ang = np.arange(T, dtype=np.float32)[:, None] * inv[None, :].astype(np.float32)
    cs, sn = np.cos(ang).astype(f).T, np.sin(ang).astype(f).T
    rc = np.ones((96, T), f)
    rs = np.zeros((96, T), f)
    rc[64:80] = cs
    rc[80:96] = cs
    rs[64:80] = sn
    rs[80:96] = sn
    shared["ropeC"] = rc
    shared["ropeS"] = rs
    return shared


def kernel(**inputs):
    inputs = {k_: np.asarray(v) for k_, v in inputs.items()}
    shared = host_prep(inputs)
    nc = build()
    x = inputs["x"]
    in_maps = []
    for b in range(8):
        m = dict(shared)
        m["xT"] = np.ascontiguousarray(x[b].T, dtype=np.float32)
        in_maps.append(m)
    res = run_bass_kernel_spmd(nc, in_maps, core_ids=list(range(8)))
    out = np.stack([np.ascontiguousarray(res.results[b]["outT"].T) for b in range(8)])
    return out.astype(np.float32)
```
